# Optimizing a Trainium2 kernel written in Bass

```python
import jax, jax.numpy as jnp
from jax import lax
import numpy as np

D_MODEL = 1024
BATCH = 16
SEQ = 2048
DEPTH = 4

RET_HEADS = D_MODEL // 128
RET_QK_DIM = 64
RET_V_DIM = 128
RET_QK = RET_HEADS * RET_QK_DIM
RET_V = RET_HEADS * RET_V_DIM
CHUNK = 128
ROPE_BASE = 10000.0
POOL_WINDOWS = (2, 4, 8, 16)
POOL_GROUPS = len(POOL_WINDOWS)
POOL_DIM = D_MODEL // 2
POOL_GROUP_DIM = POOL_DIM // POOL_GROUPS
N_BRANCH = 2
IN_COLS = 2 * RET_QK + 2 * RET_V + POOL_DIM + N_BRANCH * D_MODEL
IN_SPLITS = (RET_QK, 2 * RET_QK, 2 * RET_QK + RET_V, 2 * RET_QK + 2 * RET_V,
             2 * RET_QK + 2 * RET_V + POOL_DIM)
_FF_RAW = -(-8 * D_MODEL // 3)
D_FF = -(-_FF_RAW // 256) * 256
N_MOD = 6
EPS = 1e-6

kernel_name = "hybrid_retention_pool_adaln_block"


def rmsnorm(x, w):
    xf = x.astype(jnp.float32)
    y = xf * lax.rsqrt(jnp.mean(xf * xf, axis=-1, keepdims=True) + EPS)
    return (y * w.astype(jnp.float32)).astype(x.dtype)


def head_rmsnorm(x):
    xf = x.astype(jnp.float32)
    y = xf * lax.rsqrt(jnp.mean(xf * xf, axis=-1, keepdims=True) + EPS)
    return y.astype(x.dtype)


def modulate(h, shift, scale):
    return h * (1.0 + scale[:, None, :]) + shift[:, None, :]


def rotary(x, cos, sin):
    x1, x2 = jnp.split(x, 2, axis=-1)
    return jnp.concatenate([x1 * cos - x2 * sin, x2 * cos + x1 * sin], axis=-1)


def retention_chunkwise(q, k, v):
    bsz, s, h, dk = q.shape
    dv = v.shape[-1]
    n = s // CHUNK
    dt = q.dtype
    log_g = jnp.log1p(-(2.0 ** (-5.0 - jnp.arange(h, dtype=jnp.float32))))
    idx = jnp.arange(CHUNK, dtype=jnp.float32)
    diff = idx[:, None] - idx[None, :]
    intra = jnp.where(diff >= 0, jnp.exp(log_g[:, None, None] * jnp.maximum(diff, 0.0)), 0.0).astype(dt)
    q_dec = jnp.exp(log_g[:, None] * (idx + 1.0)).astype(dt)
    k_dec = jnp.exp(log_g[:, None] * (CHUNK - 1.0 - idx)).astype(dt)
    chunk_dec = jnp.exp(log_g * CHUNK).astype(dt)
    qc = (q * (dk ** -0.5)).reshape(bsz, n, CHUNK, h, dk)
    kc = k.reshape(bsz, n, CHUNK, h, dk)
    vc = v.reshape(bsz, n, CHUNK, h, dv)
    scores = jnp.einsum('bncha,bnmha->bnhcm', qc, kc) * intra[None, None]
    inner = jnp.einsum('bnhcm,bnmhd->bnchd', scores, vc)
    kv = jnp.einsum('bnmha,hm,bnmhd->bnhad', kc, k_dec, vc)

    def step(state, kv_i):
        return state * chunk_dec[None, :, None, None] + kv_i, state

    init = jnp.zeros((bsz, h, dk, dv), dtype=kv.dtype)
    _, prev = lax.scan(step, init, jnp.moveaxis(kv, 1, 0))
    prev = jnp.moveaxis(prev, 0, 1)
    cross = jnp.einsum('bncha,hc,bnhad->bnchd', qc, q_dec, prev)
    return (inner + cross).reshape(bsz, s, h, dv)


def causal_multiscale_pool(p):
    bsz, s, _ = p.shape
    pg = p.reshape(bsz, s, POOL_GROUPS, POOL_GROUP_DIM)
    cs = jnp.cumsum(pg.astype(jnp.float32), axis=1)
    t = jnp.arange(s, dtype=jnp.float32)
    outs = []
    for g, w in enumerate(POOL_WINDOWS):
        csg = cs[:, :, g]
        lag = jnp.pad(csg[:, :s - w], ((0, 0), (w, 0), (0, 0)))
        cnt = jnp.minimum(t + 1.0, float(w))
        outs.append((csg - lag) / cnt[None, :, None])
    mean = jnp.stack(outs, axis=2).astype(p.dtype)
    return mean - pg


def setup_inputs(seed: int = 0) -> dict:
    key = jax.random.key(seed)
    ks = jax.random.split(key, 16)
    f32 = jnp.float32
    d = D_MODEL

    def w(k, shape, fan_in, mult=1.0):
        return jax.random.normal(k, shape, f32) * (mult * fan_in ** -0.5)

    return {
        "x": jax.random.normal(ks[0], (BATCH, SEQ, d), f32),
        "c": jax.random.normal(ks[1], (BATCH, d), f32),
        "w_ada": w(ks[2], (DEPTH, d, N_MOD * d), d, 0.5),
        "b_ada": 0.01 * jax.random.normal(ks[3], (DEPTH, N_MOD * d), f32),
        "norm1": 1.0 + 0.02 * jax.random.normal(ks[4], (DEPTH, d), f32),
        "w_in": w(ks[5], (DEPTH, d, IN_COLS), d),
        "w_ret_o": w(ks[6], (DEPTH, RET_V, d), RET_V),
        "w_pool_grp": w(ks[7], (DEPTH, POOL_GROUPS, POOL_GROUP_DIM, POOL_GROUP_DIM), POOL_GROUP_DIM),
        "pool_scale": 1.0 + 0.1 * jax.random.normal(ks[8], (DEPTH, POOL_DIM), f32),
        "w_pool_o": w(ks[9], (DEPTH, POOL_DIM, d), POOL_DIM),
        "w_out": w(ks[10], (DEPTH, d, d), d),
        "norm2": 1.0 + 0.02 * jax.random.normal(ks[11], (DEPTH, d), f32),
        "w_ffn_in": w(ks[12], (DEPTH, d, 2 * D_FF), d),
        "w_ffn_out": w(ks[13], (DEPTH, D_FF, d), D_FF),
        "final_norm": 1.0 + 0.02 * jax.random.normal(ks[14], (d,), f32),
    }


def reference(x, c, w_ada, b_ada, norm1, w_in, w_ret_o, w_pool_grp, pool_scale, w_pool_o,
              w_out, norm2, w_ffn_in, w_ffn_out, final_norm):
    bsz, s, _ = x.shape
    half = RET_QK_DIM // 2
    inv_freq = ROPE_BASE ** (-jnp.arange(half, dtype=jnp.float32) / half)
    ang = jnp.arange(s, dtype=jnp.float32)[:, None] * inv_freq[None, :]
    cos = jnp.cos(ang)[:, None, :].astype(x.dtype)
    sin = jnp.sin(ang)[:, None, :].astype(x.dtype)
    c_act = jax.nn.silu(c)

    for l in range(DEPTH):
        mod = c_act @ w_ada[l] + b_ada[l]
        sh1, sc1, g1, sh2, sc2, g2 = jnp.split(mod, N_MOD, axis=-1)

        h = modulate(rmsnorm(x, norm1[l]), sh1, sc1)
        proj = h @ w_in[l]
        q, k, v, g_sw, p_in, gates = jnp.split(proj, IN_SPLITS, axis=-1)
        q = rotary(q.reshape(bsz, s, RET_HEADS, RET_QK_DIM), cos, sin)
        k = rotary(k.reshape(bsz, s, RET_HEADS, RET_QK_DIM), cos, sin)
        v = v.reshape(bsz, s, RET_HEADS, RET_V_DIM)
        ret = head_rmsnorm(retention_chunkwise(q, k, v)).reshape(bsz, s, RET_V)
        ret_d = (jax.nn.silu(g_sw) * ret) @ w_ret_o[l]

        pooled = causal_multiscale_pool(p_in)
        pooled = jnp.einsum('bsgi,gio->bsgo', pooled, w_pool_grp[l]).reshape(bsz, s, POOL_DIM)
        pool_d = (pooled * pool_scale[l]) @ w_pool_o[l]

        a_ret, a_pool = jnp.split(gates, N_BRANCH, axis=-1)
        merged = jax.nn.sigmoid(a_ret) * ret_d + jax.nn.sigmoid(a_pool) * pool_d
        x = x + g1[:, None, :] * (merged @ w_out[l])

        h2 = modulate(rmsnorm(x, norm2[l]), sh2, sc2)
        gate, up = jnp.split(h2 @ w_ffn_in[l], 2, axis=-1)
        x = x + g2[:, None, :] * ((jax.nn.silu(gate) * up) @ w_ffn_out[l])

    return rmsnorm(x, final_norm)
```

```python
import contextlib
import numpy as np
import concourse.bass as bass
import concourse.mybir as mybir
from concourse.bass_utils import run_bass_kernel_spmd

F32 = mybir.dt.float32
BF16 = mybir.dt.bfloat16
AF = mybir.ActivationFunctionType
ALU = mybir.AluOpType

D = 1024
SEQ = 2048
BATCH = 16
DEPTH = 4
NCORES = 8
TT = 1024
NSUB = 2
NCH = 8
DFF = 2816
NFF = 22
EPS = 1e-6
BLK_TILES = 8
BLK = BLK_TILES * 128
NS = 3
NW = 8
LOOKAHEAD = 2
WINDOW = NW - LOOKAHEAD
NSCR = 11
SCRW = 528
EPOCH = 12000
DEBUG_STOP = 0
DEBUG_VAR = 0

OFF = {}
_o = 0
for _name, _n in (("q", 32), ("k", 32), ("v", 64), ("g", 64), ("p", 32), ("grp", 4),
                  ("mrg", 8 * 28), ("out", 64), ("ffi", NFF * 16), ("ffo", 8 * NFF)):
    OFF[_name] = _o
    _o += _n
NT_LAYER = _o
NB_LAYER = -(-NT_LAYER // BLK_TILES)
assert OFF["v"] % 4 == 0


def _tiles_of(W):
    K, N = W.shape
    return W.reshape(K // 128, 128, N // 128, 128).transpose(2, 0, 1, 3)


def pack_layer(w_in, w_ret_o, w_pool_grp, w_pool_o, w_out, w_ffn_in, w_ffn_out):
    T = np.zeros((NB_LAYER * BLK_TILES, 128, 128), np.float32)
    tin = _tiles_of(w_in)
    T[OFF["q"]:OFF["q"] + 32] = tin[0:4].reshape(32, 128, 128)
    T[OFF["k"]:OFF["k"] + 32] = tin[4:8].reshape(32, 128, 128)
    tv = tin[8:16].reshape(2, 4, 8, 128, 128).transpose(0, 2, 1, 3, 4)
    T[OFF["v"]:OFF["v"] + 64] = tv.reshape(64, 128, 128)
    T[OFF["g"]:OFF["g"] + 64] = tin[16:24].reshape(64, 128, 128)
    T[OFF["p"]:OFF["p"] + 32] = tin[24:28].reshape(32, 128, 128)
    T[OFF["grp"]:OFF["grp"] + 4] = w_pool_grp
    tro = _tiles_of(w_ret_o)
    tpo = _tiles_of(w_pool_o)
    m = OFF["mrg"]
    for j in range(8):
        T[m:m + 8] = tro[j]
        T[m + 8:m + 12] = tpo[j]
        T[m + 12:m + 20] = tin[28 + j]
        T[m + 20:m + 28] = tin[36 + j]
        m += 28
    T[OFF["out"]:OFF["out"] + 64] = _tiles_of(w_out).reshape(64, 128, 128)
    tfi = _tiles_of(w_ffn_in)
    f = OFF["ffi"]
    for i in range(NFF):
        T[f:f + 8] = tfi[i]
        T[f + 8:f + 16] = tfi[NFF + i]
        f += 16
    T[OFF["ffo"]:OFF["ffo"] + 8 * NFF] = _tiles_of(w_ffn_out).reshape(8 * NFF, 128, 128)
    return T.reshape(NB_LAYER, BLK_TILES, 128, 128).transpose(0, 2, 1, 3).reshape(NB_LAYER, 128, BLK)


CO = {}
_c = 0
for _name, _n in (("mask", 1024), ("qdec", 512), ("kdec", 8), ("cdec", 4), ("invcnt", 64),
                  ("ident", 128), ("pswap", 128), ("onesd", 128), ("onesh", 128)):
    CO[_name] = _c
    _c += _n
NCONST = _c


def make_consts():
    C = np.zeros((128, NCONST), np.float64)
    gam = 1.0 - 2.0 ** (-5.0 - np.arange(8))
    p = np.arange(128)
    idx = np.arange(128)
    msk = np.zeros((128, 8, 128))
    for h in range(8):
        msk[:, h, :] = (idx[None, :] >= idx[:, None]) * (gam[h] ** (-(idx[:, None] + 1.0)))
    C[:, CO["mask"]:CO["mask"] + 1024] = msk.reshape(128, 1024)
    qd = np.zeros((128, 4, 128))
    for j in range(4):
        for half in range(2):
            qd[half * 64:(half + 1) * 64, j, :] = (64 ** -0.5) * gam[2 * j + half] ** (idx[None, :] + 1.0)
    C[:, CO["qdec"]:CO["qdec"] + 512] = qd.reshape(128, 512)
    for h in range(8):
        C[:, CO["kdec"] + h] = gam[h] ** (127.0 - p)
    for j in range(4):
        C[0:64, CO["cdec"] + j] = gam[2 * j] ** 128.0
        C[64:128, CO["cdec"] + j] = gam[2 * j + 1] ** 128.0
    for g, w in enumerate((2, 4, 8, 16)):
        C[:, CO["invcnt"] + g * 16:CO["invcnt"] + (g + 1) * 16] = 1.0 / np.minimum(np.arange(16) + 1.0, float(w))
    C[:, CO["ident"]:CO["ident"] + 128] = np.eye(128)
    C[p, CO["pswap"] + (p ^ 32)] = 1.0
    C[:, CO["onesd"]:CO["onesd"] + 128] = 1.0 / 1024.0
    C[:, CO["onesh"]:CO["onesh"] + 128] = 1.0 / 128.0
    C = C.astype(np.float32)
    half = 32
    inv_freq = (10000.0 ** (-(np.arange(half, dtype=np.float32) / half))).astype(np.float32)
    ang = (np.arange(SEQ, dtype=np.float32)[None, :] * inv_freq[:, None]).astype(np.float32)
    cos = np.cos(ang.astype(np.float64))
    sin = np.sin(ang.astype(np.float64))
    CS = np.zeros((128, 2, SEQ), np.float32)
    for pp in range(128):
        f = pp % 32
        CS[pp, 0] = cos[f]
        CS[pp, 1] = -sin[f] if (pp % 64) < 32 else sin[f]
    return C, CS


PO_ = {}
_c = 0
for _name, _n in (("bada", DEPTH * 48), ("norm1", DEPTH * 8), ("norm2", DEPTH * 8),
                  ("pscale", DEPTH * 4), ("fnorm", 8)):
    PO_[_name] = _c
    _c += _n
NPAR = _c


class Buf:
    __slots__ = ("name", "w", "r", "excl")

    def __init__(self, name, excl=False):
        self.name = name
        self.w = None
        self.r = {}
        self.excl = excl


class Op:
    __slots__ = ("eng", "fn", "deps", "sig", "sem", "val", "is_dma", "dsem", "batch")


class DSem:
    def __init__(self):
        self.h = None
        self.count = 0


class Prog:
    ENGS = ("pe", "act", "dve", "pool", "sp")

    def __init__(self):
        self.q = {e: [] for e in self.ENGS}
        self.dsems = []
        self.nops = 0

    def dsem(self):
        s = DSem()
        self.dsems.append(s)
        return s

    def _add(self, eng, fn, reads, writes, is_dma, dsem, batch):
        o = Op()
        o.eng = eng
        o.fn = fn
        o.sig = False
        o.sem = None
        o.val = 0
        o.is_dma = is_dma
        o.dsem = dsem
        o.batch = batch
        deps = {}

        def add(d):
            if d is None:
                return
            if (not d.is_dma) and (not is_dma) and d.eng == "pe" and eng == "pe":
                return
            deps[id(d)] = d

        for b in reads:
            add(b.w)
            if b.excl:
                for k_, x in b.r.items():
                    if k_ != eng:
                        add(x)
        for b in writes:
            add(b.w)
            for x in b.r.values():
                add(x)
        o.deps = list(deps.values())
        for d in o.deps:
            d.sig = True
        if is_dma:
            o.sig = True
        key = ("dma", id(o)) if is_dma else eng
        for b in writes:
            b.w = o
            b.r = {}
        for b in reads:
            b.r[key] = o
        self.q[eng].append(o)
        self.nops += 1
        return o

    def op(self, eng, fn, reads=(), writes=()):
        return self._add(eng, fn, reads, writes, False, None, None)

    def dma(self, eng, fn, reads, writes, dsem, batch=None):
        o = self._add(eng, fn, reads, writes, True, dsem, batch)
        if batch is not None:
            batch.append(o)
        return o

    def emit(self, nc, stack):
        esems = {}
        for e in self.ENGS:
            n = sum(1 for o in self.q[e] if o.sig and not o.is_dma)
            esems[e] = [stack.enter_context(nc.semaphore(f"s_{e}_{i}")) for i in range(n // EPOCH + 1)]
            c = 0
            for o in self.q[e]:
                if o.sig and not o.is_dma:
                    o.sem = esems[e][c // EPOCH]
                    o.val = c % EPOCH + 1
                    c += 1
        for i, s in enumerate(self.dsems):
            s.h = stack.enter_context(nc.semaphore(f"s_dma_{i}"))
        done_batches = set()
        for e in self.ENGS:
            for o in self.q[e]:
                if not o.is_dma:
                    continue
                o.sem = o.dsem.h
                if o.batch is None:
                    o.dsem.count += 16
                    o.val = o.dsem.count
                elif id(o.batch) not in done_batches:
                    done_batches.add(id(o.batch))
                    fin = o.dsem.count + 16 * len(o.batch)
                    o.dsem.count = fin
                    for x in o.batch:
                        x.val = fin
        block = stack.enter_context(nc.Block())

        def run(eng_name):
            def body(eng):
                seen = {}
                for o in self.q[eng_name]:
                    for d in o.deps:
                        k = id(d.sem)
                        if seen.get(k, 0) >= d.val:
                            continue
                        seen[k] = d.val
                        eng.wait_ge(d.sem, d.val)
                    ins = o.fn(eng)
                    if o.sig:
                        ins.then_inc(o.sem, 16 if o.is_dma else 1)
            return body

        block.tensor(run("pe"))
        block.scalar(run("act"))
        block.vector(run("dve"))
        block.gpsimd(run("pool"))
        block.sync(run("sp"))


def build_program(n_layers=DEPTH, passes=((0, 0), (0, 1), (1, 0), (1, 1))):
    nc = bass.Bass("TRN2", target_bir_lowering=False)
    P = Prog()
    stack = contextlib.ExitStack()
    n_blocks_total = n_layers * NB_LAYER

    xT = nc.dram_tensor("xT", [2, 8, 128, SEQ], F32, kind="ExternalInput").ap()
    cT = nc.dram_tensor("cT", [128, 8, 2], F32, kind="ExternalInput").ap()
    wst = nc.dram_tensor("wst", [DEPTH * NB_LAYER, 128, BLK], F32, kind="ExternalInput").ap()
    wada = nc.dram_tensor("wada", [DEPTH * 12, 128, 8 * 512], F32, kind="ExternalInput").ap()
    par = nc.dram_tensor("par", [128, NPAR], F32, kind="ExternalInput").ap()
    cst = nc.dram_tensor("cst", [128, NCONST], F32, kind="ExternalInput").ap()
    csd = nc.dram_tensor("csd", [128, 2, SEQ], F32, kind="ExternalInput").ap()
    outT = nc.dram_tensor("outT", [2, 8, 128, SEQ], F32, kind="ExternalOutput").ap()

    def sb(name, shape, dt):
        return stack.enter_context(nc.sbuf_tensor(name, shape, dt))

    x_sb = sb("x_sb", [128, 8, TT], F32)
    h_sb = sb("h_sb", [128, 8, TT], BF16)
    act_sb = sb("act_sb", [128, NFF, TT], BF16)
    rg_sb = sb("rg_sb", [128, 8, TT], BF16)
    scr_sb = sb("scr_sb", [128, NSCR, SCRW], F32)
    pl_sb = sb("pl_sb", [128, 3, 16 + TT], F32)
    cs_sb = sb("cs_sb", [128, 2, TT], F32)
    cst_sb = sb("cst_sb", [128, NCONST], F32)
    par_sb = sb("par_sb", [128, NPAR], F32)
    mod_sb = sb("mod_sb", [128, DEPTH, 48, 2], F32)
    a_sb = sb("a_sb", [128, DEPTH, 2, 8, 2], F32)
    cact_sb = sb("cact_sb", [128, 8, 2], F32)
    cbf_sb = sb("cbf_sb", [128, 2, 128], BF16)
    stg_sb = sb("stg_sb", [128, NS, BLK], F32)
    wbf_sb = sb("wbf_sb", [128, NW, BLK], BF16)
    st32_sb = sb("st32_sb", [128, DEPTH, 4, 128], F32)
    stb_sb = sb("stb_sb", [128, DEPTH, 2, 4, 128], BF16)
    carry_sb = sb("carry_sb", [128, DEPTH, 4, 16], F32)
    ps_t = [stack.enter_context(nc.psum_tensor(f"ps{i}", [128, 512], F32)) for i in range(8)]

    X = [[Buf(f"x{j}{s}") for s in range(NSUB)] for j in range(8)]
    H = [[Buf(f"h{j}{s}") for s in range(NSUB)] for j in range(8)]
    A = [[Buf(f"a{i}{s}") for s in range(NSUB)] for i in range(NFF)]
    RG = [[Buf(f"rg{j}{s}") for s in range(NSUB)] for j in range(8)]
    SCR = [Buf(f"scr{i}") for i in range(NSCR)]
    PL = [Buf(f"pl{i}") for i in range(3)]
    CSB = Buf("cs")
    CSTB = Buf("cst")
    PARB = Buf("par")
    MODB = Buf("mod")
    CACTB = Buf("cact")
    CBFB = Buf("cbf")
    STG = [Buf(f"stg{i}") for i in range(NS)]
    WBF = [Buf(f"wbf{i}") for i in range(NW)]
    ST32 = [Buf(f"st32_{l}") for l in range(DEPTH)]
    STB = [[Buf(f"stb_{l}_{i}") for i in range(2)] for l in range(DEPTH)]
    CARRY = [[Buf(f"carry_{l}_{g}") for g in range(4)] for l in range(DEPTH)]
    PS = [Buf(f"psb{i}", excl=True) for i in range(8)]

    state = {"scr": 0, "ps": 0}

    def scr():
        i = state["scr"]
        state["scr"] = (i + 1) % NSCR
        return scr_sb[:, i, :], SCR[i]

    def psum():
        i = state["ps"]
        state["ps"] = (i + 1) % 8
        return ps_t[i][:], PS[i]

    def cview(name, n):
        return cst_sb[:, CO[name]:CO[name] + n]

    class WS:
        def __init__(self):
            self.next_dma = 0
            self.next_cast = 0
            self.maxb = -1
            self.sems = [P.dsem() for _ in range(NS)]
            self.order = []
            self.ncast = 0

        def set_order(self, order):
            self.order = order

        def _dma(self, b):
            if b >= len(self.order):
                return
            slot = b % NS
            src = wst[self.order[b]]
            P.dma("sp", lambda e, slot=slot, src=src: e.dma_start(out=stg_sb[:, slot, :], in_=src),
                  reads=[], writes=[STG[slot]], dsem=self.sems[slot])

        def _cast(self, b):
            while self.next_dma < b + NS:
                self._dma(self.next_dma)
                self.next_dma += 1
            ss, ws = b % NS, b % NW
            eng = "pool" if (self.ncast % 2 == 0) else "act"
            self.ncast += 1
            if eng == "pool":
                P.op("pool", lambda e, ss=ss, ws=ws: e.tensor_copy(out=wbf_sb[:, ws, :], in_=stg_sb[:, ss, :]),
                     reads=[STG[ss]], writes=[WBF[ws]])
            else:
                P.op("act", lambda e, ss=ss, ws=ws: e.copy(out=wbf_sb[:, ws, :], in_=stg_sb[:, ss, :]),
                     reads=[STG[ss]], writes=[WBF[ws]])
            if self.next_dma == b + NS:
                self._dma(self.next_dma)
                self.next_dma += 1

        def touch(self, b):
            tgt = min(b + LOOKAHEAD, len(self.order) - 1)
            while self.next_cast <= tgt:
                self._cast(self.next_cast)
                self.next_cast += 1
            if b > self.maxb:
                self.maxb = b
            assert b > self.maxb - WINDOW, (b, self.maxb)

        def tile(self, base_blk, t, n=1):
            b = base_blk + t // BLK_TILES
            assert (t % BLK_TILES) + n <= BLK_TILES
            self.touch(b)
            ws = b % NW
            c0 = (t % BLK_TILES) * 128
            return wbf_sb[:, ws, c0:c0 + 128 * n], WBF[ws]

    ws = WS()
    order = []
    for (sq, hf) in passes:
        for l in range(n_layers):
            order.extend(range(l * NB_LAYER, (l + 1) * NB_LAYER))
    ws.set_order(order)

    ld_sem = P.dsem()
    ldb = []
    P.dma("sp", lambda e: e.dma_start(out=cst_sb[:], in_=cst[:, :]), [], [CSTB], ld_sem, ldb)
    P.dma("sp", lambda e: e.dma_start(out=par_sb[:], in_=par[:, :]), [], [PARB], ld_sem, ldb)
    P.dma("sp", lambda e: e.dma_start(out=cact_sb[:], in_=cT[:, :, :]), [], [CACTB], ld_sem, ldb)
    P.op("dve", lambda e: e.tensor_copy(out=cbf_sb[:, 0, :], in_=cview("ident", 128)), [CSTB], [CBFB])
    P.op("dve", lambda e: e.tensor_copy(out=cbf_sb[:, 1, :], in_=cview("pswap", 128)), [CBFB, CSTB], [CBFB])
    P.op("act", lambda e: e.activation(out=cact_sb[:], in_=cact_sb[:], func=AF.Silu), [CACTB], [CACTB])
    ident = cbf_sb[:, 0, :]
    pswap = cbf_sb[:, 1, :]
    onesd = cview("onesd", 128)
    onesh = cview("onesh", 128)

    wa_f32 = act_sb[:].rearrange("p a b -> p (a b)").bitcast(F32)
    WA = [Buf("wa0"), Buf("wa1")]
    wa_sems = [P.dsem(), P.dsem()]
    allA = [A[i][s] for i in range(NFF) for s in range(NSUB)]
    for l in range(n_layers):
        mps, mpsb = psum()
        for pc in range(12):
            slot = (l * 12 + pc) % 2
            src = wada[l * 12 + pc]
            P.dma("sp", lambda e, slot=slot, src=src: e.dma_start(out=wa_f32[:, slot * 4096:(slot + 1) * 4096], in_=src),
                  [], [WA[slot]], wa_sems[slot])
            for ct in range(4):
                t = pc * 4 + ct
                for kk in range(8):
                    P.op("pe", lambda e, slot=slot, ct=ct, kk=kk, t=t, mps=mps: e.matmul(
                        mps[:, 2 * t:2 * t + 2],
                        wa_f32[:, slot * 4096 + kk * 512 + ct * 128: slot * 4096 + kk * 512 + ct * 128 + 128],
                        cact_sb[:, kk, :], start=(kk == 0), stop=(kk == 7)),
                        [WA[slot], CACTB], [mpsb])
        bada = par_sb[:, PO_["bada"] + l * 48: PO_["bada"] + (l + 1) * 48]
        P.op("dve", lambda e, l=l, mps=mps, bada=bada: e.tensor_tensor(
            out=mod_sb[:, l, :, :], in0=mps[:, 0:96].rearrange("p (t b) -> p t b", b=2),
            in1=bada.unsqueeze(2).broadcast_to([128, 48, 2]), op=ALU.add), [mpsb, PARB], [MODB])
        for k, (t0, nm) in enumerate(((8, "norm1"), (32, "norm2"))):
            nv = par_sb[:, PO_[nm] + l * 8: PO_[nm] + (l + 1) * 8]
            P.op("dve", lambda e, l=l, k=k, t0=t0, nv=nv: e.scalar_tensor_tensor(
                out=a_sb[:, l, k, :, :], in0=mod_sb[:, l, t0:t0 + 8, :], scalar=1.0,
                in1=nv.unsqueeze(2).broadcast_to([128, 8, 2]), op0=ALU.add, op1=ALU.mult), [MODB, PARB], [MODB])
    P.op("pool", lambda e: e.memset(act_sb[:, 0, 0:16], 0.0), [], allA + WA)

    def modv(l, t, b):
        return mod_sb[:, l, t, b:b + 1]

    def rmsnorm_to_h(l, bsel, which):
        tB = 0 if which == 0 else 24
        for s in range(NSUB):
            sl = slice(s * 512, (s + 1) * 512)
            stp, stb_ = psum()
            for j in range(8):
                sq, sqb = scr()
                P.op("act", lambda e, j=j, sq=sq, sl=sl: e.activation(out=sq[:, 0:512], in_=x_sb[:, j, sl], func=AF.Square),
                     [X[j][s]], [sqb])
                P.op("pe", lambda e, j=j, sq=sq, stp=stp: e.matmul(stp, onesd, sq[:, 0:512], start=(j == 0), stop=(j == 7)),
                     [sqb, CSTB], [stb_])
            rs, rsb = scr()
            P.op("act", lambda e, rs=rs, stp=stp: e.activation(out=rs[:, 0:512], in_=stp, func=AF.Sqrt, bias=EPS, scale=1.0),
                 [stb_], [rsb])
            P.op("dve", lambda e, rs=rs: e.reciprocal(out=rs[:, 0:512], in_=rs[:, 0:512]), [rsb], [rsb])
            for j in range(8):
                tm, tmb = scr()
                P.op("dve", lambda e, j=j, tm=tm, rs=rs, sl=sl: e.tensor_tensor(
                    out=tm[:, 0:512], in0=x_sb[:, j, sl], in1=rs[:, 0:512], op=ALU.mult), [X[j][s], rsb], [tmb])
                P.op("act", lambda e, j=j, tm=tm, sl=sl: e.activation(
                    out=h_sb[:, j, sl], in_=tm[:, 0:512], func=AF.Identity,
                    bias=modv(l, tB + j, bsel), scale=a_sb[:, l, which, j, bsel:bsel + 1]),
                    [tmb, MODB], [H[j][s]])

    def proj_fm(base, t0, s, nk=8, rhs_of=None):
        ps, psb = psum()
        sl = slice(s * 512, (s + 1) * 512)
        for kk in range(nk):
            w, wb = ws.tile(base, t0 + kk)
            r, rb = rhs_of(kk, s, sl)
            P.op("pe", lambda e, ps=ps, w=w, r=r, kk=kk: e.matmul(ps, w, r, start=(kk == 0), stop=(kk == nk - 1)),
                 [wb, rb], [psb])
        return ps, psb

    def rhs_h(kk, s, sl):
        return h_sb[:, kk, sl], H[kk][s]

    def rhs_rg(kk, s, sl):
        return rg_sb[:, kk, sl], RG[kk][s]

    def rhs_a(off):
        def f(kk, s, sl):
            return act_sb[:, off + kk, sl], A[off + kk][s]
        return f

    def layer(l, bsel, hf, base):
        pos0 = hf * TT
        rmsnorm_to_h(l, bsel, 0)

        if DEBUG_STOP == 1:
            return
        def rot_stage2(part, j, s, sl, rawbf, rawb, t1, t1b):
            ps2, ps2b = psum()
            P.op("pe", lambda e: e.matmul(ps2, pswap, rawbf, start=True, stop=True), [rawb, CBFB], [ps2b])
            t2, t2b = scr()
            P.op("dve", lambda e: e.tensor_tensor(out=t2[:, 0:512], in0=ps2, in1=cs_sb[:, 1, sl], op=ALU.mult),
                 [ps2b, CSB], [t2b])
            if part == 1:
                P.op("pool", lambda e: e.tensor_tensor(
                    out=act_sb[:, 4 + j, sl], in0=t1[:, 0:512], in1=t2[:, 0:512], op=ALU.add),
                    [t1b, t2b], [A[4 + j][s]])
            else:
                P.op("pool", lambda e: e.tensor_tensor(
                    out=t1[:, 0:512], in0=t1[:, 0:512], in1=t2[:, 0:512], op=ALU.add), [t1b, t2b], [t1b])
                qd = cst_sb[:, CO["qdec"] + j * 128: CO["qdec"] + (j + 1) * 128]
                P.op("pool", lambda e: e.tensor_tensor(
                    out=act_sb[:, j, sl].rearrange("p (a b) -> p a b", a=4),
                    in0=t1[:, 0:512].rearrange("p (a b) -> p a b", a=4),
                    in1=qd.unsqueeze(1).broadcast_to([128, 4, 128]), op=ALU.mult),
                    [t1b, CSTB], [A[j][s]])

        pending = None
        for part in range(2):
            for j in range(4):
                for s in range(NSUB):
                    sl = slice(s * 512, (s + 1) * 512)
                    ps, psb = proj_fm(base, OFF["q" if part == 0 else "k"] + j * 8, s, 8, rhs_h)
                    raw, rawb = scr()
                    rawbf = raw.bitcast(BF16)[:, 0:512]
                    P.op("act", lambda e, rawbf=rawbf, ps=ps: e.copy(out=rawbf, in_=ps), [psb], [rawb])
                    t1, t1b = scr()
                    P.op("dve", lambda e, t1=t1, ps=ps, sl=sl: e.tensor_tensor(
                        out=t1[:, 0:512], in0=ps, in1=cs_sb[:, 0, sl], op=ALU.mult), [psb, CSB], [t1b])
                    if pending is not None:
                        rot_stage2(*pending)
                    pending = (part, j, s, sl, rawbf, rawb, t1, t1b)
        rot_stage2(*pending)

        kdec = cview("kdec", 8)

        def k_transpose(n):
            s, c0 = n // 4, (n % 4) * 128
            ps, psb = psum()
            psbf = ps.bitcast(BF16)
            for j in range(4):
                P.op("pe", lambda e, j=j: e.transpose(
                    psbf[:, j * 128:(j + 1) * 128], act_sb[:, 4 + j, s * 512 + c0: s * 512 + c0 + 128], ident),
                    [A[4 + j][s], CBFB], [psb])
            P.op("dve", lambda e: e.tensor_tensor(
                out=act_sb[:, 16 + n // 2, (n % 2) * 512:(n % 2 + 1) * 512].rearrange("p (h a) -> p h a", h=8),
                in0=psbf[:, 0:512].rearrange("p (h a) -> p h a", h=8),
                in1=kdec.unsqueeze(2).broadcast_to([128, 8, 64]), op=ALU.mult),
                [psb, CSTB], [A[16 + n // 2][n % 2]])

        gi = 0
        for cg in range(2):
            for n in range(NCH):
                s, c0 = n // 4, (n % 4) * 128
                ps, psb = psum()
                for kk in range(8):
                    w, wb = ws.tile(base, OFF["v"] + (cg * 8 + kk) * 4, 4)
                    P.op("pe", lambda e, ps=ps, w=w, kk=kk, s=s, c0=c0: e.matmul(
                        ps, h_sb[:, kk, s * 512 + c0: s * 512 + c0 + 128], w, start=(kk == 0), stop=(kk == 7)),
                        [wb, H[kk][s]], [psb])
                P.op("dve", lambda e, ps=ps, n=n, cg=cg: e.tensor_copy(out=act_sb[:, 8 + n, cg * 512:(cg + 1) * 512], in_=ps),
                     [psb], [A[8 + n][cg]])
                if gi % 2 == 1:
                    k_transpose(gi // 2)
                gi += 1

        if hf == 0:
            P.op("pool", lambda e: e.memset(st32_sb[:, l, :, :], 0.0), [], [ST32[l]])
            P.op("pool", lambda e: e.memset(stb_sb[:, l, 0, :, :], 0.0), [], [STB[l][0]])

        mask = cview("mask", 1024)
        cdec = cview("cdec", 4)
        mask4 = mask.rearrange("p (j q c) -> p j q c", j=4, q=2)
        rg4 = rg_sb[:].rearrange("p (j q) t -> p j q t", q=2)
        for n in range(NCH):
            s, c0 = n // 4, (n % 4) * 128
            tsl = slice(s * 512 + c0, s * 512 + c0 + 128)
            vbufs = [A[8 + n][0], A[8 + n][1]]
            cur, nxt = n % 2, (n + 1) % 2
            sc_ps = [psum(), psum()]
            for h in range(8):
                j, par = h // 2, h % 2
                p0 = par * 64
                bank, bankb = sc_ps[par]
                P.op("pe", lambda e, bank=bank, j=j, p0=p0, tsl=tsl: e.matmul(
                    bank[:, j * 128:(j + 1) * 128], act_sb[p0:p0 + 64, 4 + j, tsl], act_sb[p0:p0 + 64, j, tsl],
                    start=True, stop=True), [A[4 + j][s], A[j][s]], [bankb])
            pkv, pkvb = psum()
            kdb = A[16 + n // 2][n % 2]
            for h in range(8):
                P.op("pe", lambda e, pkv=pkv, h=h, n=n: e.matmul(
                    pkv[(h % 2) * 64:(h % 2) * 64 + 64, (h // 2) * 128:(h // 2 + 1) * 128],
                    act_sb[:, 16 + n // 2, (n % 2) * 512 + h * 64:(n % 2) * 512 + (h + 1) * 64],
                    act_sb[:, 8 + n, h * 128:(h + 1) * 128], start=True, stop=True),
                    [kdb, vbufs[h // 4]], [pkvb])
            stts = []
            for par in range(2):
                bank, bankb = sc_ps[par]
                stt, sttb = scr()
                sttbf = stt.bitcast(BF16)[:, 0:512]
                P.op("dve", lambda e, sttbf=sttbf, bank=bank, par=par: e.tensor_tensor(
                    out=sttbf.rearrange("p (j c) -> p j c", j=4), in0=bank.rearrange("p (j c) -> p j c", j=4),
                    in1=mask4[:, :, par, :], op=ALU.mult), [bankb, CSTB], [sttb])
                stts.append((sttbf, sttb))
            tmp, tmpb = scr()
            P.op("pool", lambda e, tmp=tmp: e.tensor_tensor(
                out=tmp[:, 0:512].rearrange("p (a b) -> p a b", a=4), in0=st32_sb[:, l, :, :],
                in1=cdec.unsqueeze(2).broadcast_to([128, 4, 128]), op=ALU.mult), [ST32[l], CSTB], [tmpb])
            P.op("dve", lambda e, tmp=tmp, pkv=pkv: e.tensor_tensor(
                out=st32_sb[:, l, :, :], in0=tmp[:, 0:512].rearrange("p (a b) -> p a b", a=4),
                in1=pkv.rearrange("p (a b) -> p a b", a=4), op=ALU.add), [tmpb, pkvb], [ST32[l]])
            P.op("act", lambda e, nxt=nxt: e.copy(out=stb_sb[:, l, nxt, :, :], in_=st32_sb[:, l, :, :]),
                 [ST32[l]], [STB[l][nxt]])
            o_ps = [psum(), psum()]
            for h in range(8):
                j, par = h // 2, h % 2
                p0 = par * 64
                bank, bankb = o_ps[par]
                sttbf, sttb = stts[par]
                P.op("pe", lambda e, bank=bank, j=j, h=h, n=n, sttbf=sttbf: e.matmul(
                    bank[:, j * 128:(j + 1) * 128], act_sb[:, 8 + n, h * 128:(h + 1) * 128],
                    sttbf[:, j * 128:(j + 1) * 128], start=True, stop=False),
                    [vbufs[h // 4], sttb], [bankb])
                P.op("pe", lambda e, bank=bank, j=j, p0=p0, tsl=tsl, cur=cur: e.matmul(
                    bank[:, j * 128:(j + 1) * 128], stb_sb[p0:p0 + 64, l, cur, j, :],
                    act_sb[p0:p0 + 64, j, tsl], start=False, stop=True),
                    [STB[l][cur], A[j][s]], [bankb])
            for par in range(2):
                bank, bankb = o_ps[par]
                osq, osqb = scr()
                P.op("act", lambda e, osq=osq, bank=bank: e.activation(out=osq[:, 0:512], in_=bank, func=AF.Square),
                     [bankb], [osqb])
                psn, psnb = psum()
                P.op("pe", lambda e, psn=psn, osq=osq: e.matmul(psn, onesh, osq[:, 0:512], start=True, stop=True),
                     [osqb, CSTB], [psnb])
                hr, hrb = scr()
                P.op("act", lambda e, hr=hr, psn=psn: e.activation(out=hr[:, 0:512], in_=psn, func=AF.Sqrt, bias=EPS, scale=1.0),
                     [psnb], [hrb])
                P.op("dve", lambda e, hr=hr: e.reciprocal(out=hr[:, 0:512], in_=hr[:, 0:512]), [hrb], [hrb])
                P.op("dve", lambda e, hr=hr, bank=bank, par=par, tsl=tsl: e.tensor_tensor(
                    out=rg4[:, :, par, tsl], in0=bank.rearrange("p (a b) -> p a b", a=4),
                    in1=hr[:, 0:512].rearrange("p (a b) -> p a b", a=4), op=ALU.mult),
                    [hrb, bankb], [RG[2 * i + par][s] for i in range(4)])

        if DEBUG_STOP == 5:
            return
        for h in range(8):
            for s in range(NSUB):
                sl = slice(s * 512, (s + 1) * 512)
                ps, psb = proj_fm(base, OFF["g"] + h * 8, s, 8, rhs_h)
                sg, sgb = scr()
                sgbf = sg.bitcast(BF16)[:, 0:512]
                P.op("act", lambda e, sgbf=sgbf, ps=ps: e.activation(out=sgbf, in_=ps, func=AF.Silu), [psb], [sgb])
                P.op("pool", lambda e, h=h, sl=sl, sgbf=sgbf: e.tensor_tensor(
                    out=rg_sb[:, h, sl], in0=rg_sb[:, h, sl], in1=sgbf, op=ALU.mult), [sgb, RG[h][s]], [RG[h][s]])

        if DEBUG_STOP == 6:
            return
        invc = cview("invcnt", 64)
        for g in range(4):
            w = 2 << g
            for s in range(NSUB):
                ps, psb = proj_fm(base, OFF["p"] + g * 8, s, 8, rhs_h)
                P.op("act", lambda e, ps=ps, s=s: e.copy(out=pl_sb[:, 0, 16 + s * 512:16 + (s + 1) * 512], in_=ps),
                     [psb], [PL[0]])
            if hf == 0:
                P.op("pool", lambda e: e.memset(pl_sb[:, 0, 0:16], 0.0), [], [PL[0]])
            else:
                P.op("pool", lambda e, g=g: e.tensor_copy(out=pl_sb[:, 0, 0:16], in_=carry_sb[:, l, g, :]),
                     [CARRY[l][g]], [PL[0]])
            P.op("pool", lambda e, g=g: e.tensor_copy(out=carry_sb[:, l, g, :], in_=pl_sb[:, 0, TT:TT + 16]),
                 [PL[0]], [CARRY[l][g]])
            cur, curb = 0, PL[0]
            sh, lo = 1, 0
            while sh < w:
                nxt = 1 if cur != 1 else 2
                lo2 = lo + sh
                P.op("pool", lambda e, cur=cur, nxt=nxt, sh=sh, lo2=lo2: e.tensor_tensor(
                    out=pl_sb[:, nxt, lo2:16 + TT], in0=pl_sb[:, cur, lo2:16 + TT],
                    in1=pl_sb[:, cur, lo2 - sh:16 + TT - sh], op=ALU.add), [PL[cur]], [PL[nxt]])
                cur, lo, sh = nxt, lo2, sh * 2
            P.op("dve", lambda e, cur=cur, g=g, w=w: e.scalar_tensor_tensor(
                out=act_sb[:, 8 + g, :], in0=pl_sb[:, cur, 16:16 + TT], scalar=1.0 / w,
                in1=pl_sb[:, 0, 16:16 + TT], op0=ALU.mult, op1=ALU.subtract), [PL[cur], PL[0]], [A[8 + g][0], A[8 + g][1]])
            if hf == 0:
                tm, tmb = scr()
                P.op("dve", lambda e, tm=tm, cur=cur, g=g: e.tensor_tensor(
                    out=tm[:, 0:16], in0=pl_sb[:, cur, 16:32], in1=invc[:, g * 16:(g + 1) * 16], op=ALU.mult),
                    [PL[cur], CSTB], [tmb])
                P.op("dve", lambda e, tm=tm, g=g: e.tensor_tensor(
                    out=act_sb[:, 8 + g, 0:16], in0=tm[:, 0:16], in1=pl_sb[:, 0, 16:32], op=ALU.subtract),
                    [tmb, PL[0], A[8 + g][0]], [A[8 + g][0]])
        for g in range(4):
            for s in range(NSUB):
                sl = slice(s * 512, (s + 1) * 512)
                ps, psb = psum()
                wt, wtb = ws.tile(base, OFF["grp"] + g)
                P.op("pe", lambda e, ps=ps, wt=wt, g=g, sl=sl: e.matmul(ps, wt, act_sb[:, 8 + g, sl], start=True, stop=True),
                     [wtb, A[8 + g][s]], [psb])
                psc = par_sb[:, PO_["pscale"] + l * 4 + g: PO_["pscale"] + l * 4 + g + 1]
                P.op("act", lambda e, ps=ps, g=g, sl=sl, psc=psc: e.activation(
                    out=act_sb[:, 12 + g, sl], in_=ps, func=AF.Identity, bias=0.0, scale=psc), [psb, PARB], [A[12 + g][s]])

        if DEBUG_STOP == 7:
            return
        for j in range(8):
            m0 = OFF["mrg"] + j * 28
            for s in range(NSUB):
                sl = slice(s * 512, (s + 1) * 512)
                prd, prdb = proj_fm(base, m0, s, 8, rhs_rg)
                ppd, ppdb = proj_fm(base, m0 + 8, s, 4, rhs_a(12))
                par_, parb_ = proj_fm(base, m0 + 12, s, 8, rhs_h)
                pap, papb = proj_fm(base, m0 + 20, s, 8, rhs_h)
                sr, srb = scr()
                P.op("act", lambda e, sr=sr, par_=par_: e.activation(out=sr[:, 0:512], in_=par_, func=AF.Sigmoid), [parb_], [srb])
                sp_, spb = scr()
                P.op("act", lambda e, sp_=sp_, pap=pap: e.activation(out=sp_[:, 0:512], in_=pap, func=AF.Sigmoid), [papb], [spb])
                P.op("dve", lambda e, sr=sr, prd=prd: e.tensor_tensor(out=sr[:, 0:512], in0=prd, in1=sr[:, 0:512], op=ALU.mult),
                     [prdb, srb], [srb])
                P.op("dve", lambda e, sp_=sp_, ppd=ppd: e.tensor_tensor(out=sp_[:, 0:512], in0=ppd, in1=sp_[:, 0:512], op=ALU.mult),
                     [ppdb, spb], [spb])
                P.op("pool", lambda e, j=j, sl=sl, sr=sr, sp_=sp_: e.tensor_tensor(
                    out=act_sb[:, j, sl], in0=sr[:, 0:512], in1=sp_[:, 0:512], op=ALU.add), [srb, spb], [A[j][s]])

        if DEBUG_STOP == 8:
            return
        for j in range(8):
            for s in range(NSUB):
                sl = slice(s * 512, (s + 1) * 512)
                ps, psb = proj_fm(base, OFF["out"] + j * 8, s, 8, rhs_a(0))
                P.op("dve", lambda e, ps=ps, j=j, sl=sl: e.scalar_tensor_tensor(
                    out=x_sb[:, j, sl], in0=ps, scalar=modv(l, 16 + j, bsel), in1=x_sb[:, j, sl],
                    op0=ALU.mult, op1=ALU.add), [psb, MODB, X[j][s]], [X[j][s]])

        if DEBUG_STOP == 9:
            return
        rmsnorm_to_h(l, bsel, 1)
        for i in range(NFF):
            for s in range(NSUB):
                sl = slice(s * 512, (s + 1) * 512)
                pg, pgb = proj_fm(base, OFF["ffi"] + i * 16, s, 8, rhs_h)
                pu, pub = proj_fm(base, OFF["ffi"] + i * 16 + 8, s, 8, rhs_h)
                sg, sgb = scr()
                P.op("act", lambda e, sg=sg, pg=pg: e.activation(out=sg[:, 0:512], in_=pg, func=AF.Silu), [pgb], [sgb])
                P.op("dve", lambda e, sg=sg, pu=pu, i=i, sl=sl: e.tensor_tensor(
                    out=act_sb[:, i, sl], in0=pu, in1=sg[:, 0:512], op=ALU.mult), [pub, sgb], [A[i][s]])
        for j in range(8):
            for s in range(NSUB):
                sl = slice(s * 512, (s + 1) * 512)
                ps, psb = proj_fm(base, OFF["ffo"] + j * NFF, s, NFF, rhs_a(0))
                P.op("dve", lambda e, ps=ps, j=j, sl=sl: e.scalar_tensor_tensor(
                    out=x_sb[:, j, sl], in0=ps, scalar=modv(l, 40 + j, bsel), in1=x_sb[:, j, sl],
                    op0=ALU.mult, op1=ALU.add), [psb, MODB, X[j][s]], [X[j][s]])

    xl_sem = P.dsem()
    st_sem = P.dsem()
    cs_sem = P.dsem()
    store_ops = []
    blk = 0
    for (sq, hf) in passes:
        pos0 = hf * TT
        lb = []
        for j in range(8):
            for s in range(NSUB):
                P.dma("act", lambda e, j=j, s=s, sq=sq, pos0=pos0: e.dma_start(
                    out=x_sb[:, j, s * 512:(s + 1) * 512], in_=xT[sq, j, :, pos0 + s * 512: pos0 + (s + 1) * 512]),
                    [], [X[j][s]], xl_sem, lb)
        P.dma("act", lambda e, pos0=pos0: e.dma_start(out=cs_sb[:], in_=csd[:, :, pos0:pos0 + TT]), [], [CSB], cs_sem)
        for l in range(n_layers):
            layer(l, sq, hf, blk)
            blk += NB_LAYER
        fn = par_sb[:, PO_["fnorm"]: PO_["fnorm"] + 8]
        sbatch = []
        for s in range(NSUB):
            sl = slice(s * 512, (s + 1) * 512)
            stp, stb_ = psum()
            for j in range(8):
                sq_, sqb = scr()
                P.op("act", lambda e, j=j, sq_=sq_, sl=sl: e.activation(out=sq_[:, 0:512], in_=x_sb[:, j, sl], func=AF.Square),
                     [X[j][s]], [sqb])
                P.op("pe", lambda e, j=j, sq_=sq_, stp=stp: e.matmul(stp, onesd, sq_[:, 0:512], start=(j == 0), stop=(j == 7)),
                     [sqb, CSTB], [stb_])
            rs, rsb = scr()
            P.op("act", lambda e, rs=rs, stp=stp: e.activation(out=rs[:, 0:512], in_=stp, func=AF.Sqrt, bias=EPS, scale=1.0),
                 [stb_], [rsb])
            P.op("dve", lambda e, rs=rs: e.reciprocal(out=rs[:, 0:512], in_=rs[:, 0:512]), [rsb], [rsb])
            for j in range(8):
                P.op("dve", lambda e, j=j, rs=rs, sl=sl: e.scalar_tensor_tensor(
                    out=x_sb[:, j, sl], in0=x_sb[:, j, sl], scalar=fn[:, j:j + 1], in1=rs[:, 0:512],
                    op0=ALU.mult, op1=ALU.mult), [X[j][s], rsb, PARB], [X[j][s]])
                o = P.dma("act", lambda e, j=j, s=s, sq=sq, pos0=pos0, sl=sl: e.dma_start(
                    out=outT[sq, j, :, pos0 + s * 512: pos0 + (s + 1) * 512], in_=x_sb[:, j, sl]),
                    [X[j][s]], [], st_sem, sbatch)
                store_ops.append(o)
    fin = P.op("act", lambda e: e.activation(out=scr_sb[:, 0, 0:8], in_=scr_sb[:, 0, 0:8], func=AF.Copy), [], [SCR[0]])
    dd = {id(o): o for o in fin.deps}
    for o in store_ops:
        dd[id(o)] = o
    fin.deps = list(dd.values())

    P.emit(nc, stack)
    return nc, stack, P


def prep_shared(w_ada, b_ada, norm1, w_in, w_ret_o, w_pool_grp, pool_scale, w_pool_o,
                w_out, norm2, w_ffn_in, w_ffn_out, final_norm):
    f = lambda a: np.ascontiguousarray(np.asarray(a, dtype=np.float32))
    wst = np.concatenate([pack_layer(f(w_in[l]), f(w_ret_o[l]), f(w_pool_grp[l]), f(w_pool_o[l]), f(w_out[l]),
                                     f(w_ffn_in[l]), f(w_ffn_out[l])) for l in range(DEPTH)], axis=0)
    wa = f(w_ada).reshape(DEPTH, 8, 128, 12, 512).transpose(0, 3, 2, 1, 4).reshape(DEPTH * 12, 128, 8 * 512)
    par = np.zeros((128, NPAR), np.float32)
    par[:, PO_["bada"]:PO_["bada"] + DEPTH * 48] = f(b_ada).reshape(DEPTH, 48, 128).transpose(2, 0, 1).reshape(128, -1)
    par[:, PO_["norm1"]:PO_["norm1"] + DEPTH * 8] = f(norm1).reshape(DEPTH, 8, 128).transpose(2, 0, 1).reshape(128, -1)
    par[:, PO_["norm2"]:PO_["norm2"] + DEPTH * 8] = f(norm2).reshape(DEPTH, 8, 128).transpose(2, 0, 1).reshape(128, -1)
    par[:, PO_["pscale"]:PO_["pscale"] + DEPTH * 4] = f(pool_scale).reshape(DEPTH, 4, 128).transpose(2, 0, 1).reshape(128, -1)
    par[:, PO_["fnorm"]:PO_["fnorm"] + 8] = f(final_norm).reshape(8, 128).T
    cst, csd = make_consts()
    return {"wst": np.ascontiguousarray(wst), "wada": np.ascontiguousarray(wa), "par": par, "cst": cst, "csd": csd}


def prep_core(x, c, core):
    xs = np.asarray(x[2 * core:2 * core + 2], dtype=np.float32)
    xT = np.ascontiguousarray(xs.transpose(0, 2, 1).reshape(2, 8, 128, SEQ))
    cs = np.asarray(c[2 * core:2 * core + 2], dtype=np.float32)
    cT = np.ascontiguousarray(cs.reshape(2, 8, 128).transpose(2, 1, 0))
    return {"xT": xT, "cT": cT}


_CACHE = {}


def kernel(x, c, w_ada, b_ada, norm1, w_in, w_ret_o, w_pool_grp, pool_scale, w_pool_o,
           w_out, norm2, w_ffn_in, w_ffn_out, final_norm):
    shared = prep_shared(w_ada, b_ada, norm1, w_in, w_ret_o, w_pool_grp, pool_scale, w_pool_o,
                         w_out, norm2, w_ffn_in, w_ffn_out, final_norm)
    nc, stack, P = build_program()
    in_maps = []
    for core in range(NCORES):
        m = dict(shared)
        m.update(prep_core(x, c, core))
        in_maps.append(m)
    res = run_bass_kernel_spmd(nc, in_maps, core_ids=list(range(NCORES)))
    out = np.empty((BATCH, SEQ, D), np.float32)
    for core in range(NCORES):
        o = np.asarray(res.results[core]["outT"]).reshape(2, D, SEQ)
        out[2 * core:2 * core + 2] = o.transpose(0, 2, 1)
    return out
```

```python
import contextlib
import numpy as np
import concourse.bass as bass
import concourse.mybir as mybir
from concourse.bass_utils import run_bass_kernel_spmd

F32 = mybir.dt.float32
BF16 = mybir.dt.bfloat16
AF = mybir.ActivationFunctionType
ALU = mybir.AluOpType

D = 1024
SEQ = 2048
BATCH = 16
DEPTH = 4
NCORES = 8
TT = 1024
NSUB = 2
NCH = 8
DFF = 2816
NFF = 22
EPS = 1e-6
BLK_TILES = 8
BLK = BLK_TILES * 128
NS = 3
NW = 8
LOOKAHEAD = 3
WINDOW = NW - LOOKAHEAD
NSCR = 11
SCRW = 528
EPOCH = 12000
DEBUG_STOP = 0
DEBUG_VAR = 0

OFF = {}
_o = 0
for _name, _n in (("q", 32), ("k", 32), ("v", 64), ("g", 64), ("p", 32), ("grp", 4),
                  ("mrg", 8 * 28), ("out", 64), ("ffi", NFF * 16), ("ffo", 8 * NFF)):
    OFF[_name] = _o
    _o += _n
NT_LAYER = _o
NB_LAYER = -(-NT_LAYER // BLK_TILES)
assert OFF["v"] % 4 == 0


def _tiles_of(W):
    K, N = W.shape
    return W.reshape(K // 128, 128, N // 128, 128).transpose(2, 0, 1, 3)


def pack_layer(w_in, w_ret_o, w_pool_grp, w_pool_o, w_out, w_ffn_in, w_ffn_out):
    T = np.zeros((NB_LAYER * BLK_TILES, 128, 128), np.float32)
    tin = _tiles_of(w_in)
    T[OFF["q"]:OFF["q"] + 32] = tin[0:4].reshape(32, 128, 128)
    T[OFF["k"]:OFF["k"] + 32] = tin[4:8].reshape(32, 128, 128)
    tv = tin[8:16].reshape(2, 4, 8, 128, 128).transpose(0, 2, 1, 3, 4)
    T[OFF["v"]:OFF["v"] + 64] = tv.reshape(64, 128, 128)
    T[OFF["g"]:OFF["g"] + 64] = tin[16:24].reshape(64, 128, 128)
    T[OFF["p"]:OFF["p"] + 32] = tin[24:28].reshape(32, 128, 128)
    T[OFF["grp"]:OFF["grp"] + 4] = w_pool_grp
    tro = _tiles_of(w_ret_o)
    tpo = _tiles_of(w_pool_o)
    m = OFF["mrg"]
    for j in range(8):
        T[m:m + 8] = tro[j]
        T[m + 8:m + 12] = tpo[j]
        T[m + 12:m + 20] = tin[28 + j]
        T[m + 20:m + 28] = tin[36 + j]
        m += 28
    T[OFF["out"]:OFF["out"] + 64] = _tiles_of(w_out).reshape(64, 128, 128)
    tfi = _tiles_of(w_ffn_in)
    f = OFF["ffi"]
    for i in range(NFF):
        T[f:f + 8] = tfi[i]
        T[f + 8:f + 16] = tfi[NFF + i]
        f += 16
    T[OFF["ffo"]:OFF["ffo"] + 8 * NFF] = _tiles_of(w_ffn_out).reshape(8 * NFF, 128, 128)
    return T.reshape(NB_LAYER, BLK_TILES, 128, 128).transpose(0, 2, 1, 3).reshape(NB_LAYER, 128, BLK)


CO = {}
_c = 0
for _name, _n in (("mask", 1024), ("qdec", 512), ("kdec", 8), ("cdec", 4), ("invcnt", 64),
                  ("ident", 128), ("pswap", 128), ("onesd", 128), ("onesh", 128)):
    CO[_name] = _c
    _c += _n
NCONST = _c


def make_consts():
    C = np.zeros((128, NCONST), np.float64)
    gam = 1.0 - 2.0 ** (-5.0 - np.arange(8))
    p = np.arange(128)
    idx = np.arange(128)
    msk = np.zeros((128, 8, 128))
    for h in range(8):
        msk[:, h, :] = (idx[None, :] >= idx[:, None]) * (gam[h] ** (-(idx[:, None] + 1.0)))
    C[:, CO["mask"]:CO["mask"] + 1024] = msk.reshape(128, 1024)
    qd = np.zeros((128, 4, 128))
    for j in range(4):
        for half in range(2):
            qd[half * 64:(half + 1) * 64, j, :] = (64 ** -0.5) * gam[2 * j + half] ** (idx[None, :] + 1.0)
    C[:, CO["qdec"]:CO["qdec"] + 512] = qd.reshape(128, 512)
    for h in range(8):
        C[:, CO["kdec"] + h] = gam[h] ** (127.0 - p)
    for j in range(4):
        C[0:64, CO["cdec"] + j] = gam[2 * j] ** 128.0
        C[64:128, CO["cdec"] + j] = gam[2 * j + 1] ** 128.0
    for g, w in enumerate((2, 4, 8, 16)):
        C[:, CO["invcnt"] + g * 16:CO["invcnt"] + (g + 1) * 16] = 1.0 / np.minimum(np.arange(16) + 1.0, float(w))
    C[:, CO["ident"]:CO["ident"] + 128] = np.eye(128)
    C[p, CO["pswap"] + (p ^ 32)] = 1.0
    C[:, CO["onesd"]:CO["onesd"] + 128] = 1.0 / 1024.0
    C[:, CO["onesh"]:CO["onesh"] + 128] = 1.0 / 128.0
    C = C.astype(np.float32)
    half = 32
    inv_freq = (10000.0 ** (-(np.arange(half, dtype=np.float32) / half))).astype(np.float32)
    ang = (np.arange(SEQ, dtype=np.float32)[None, :] * inv_freq[:, None]).astype(np.float32)
    cos = np.cos(ang.astype(np.float64))
    sin = np.sin(ang.astype(np.float64))
    CS = np.zeros((128, 2, SEQ), np.float32)
    for pp in range(128):
        f = pp % 32
        CS[pp, 0] = cos[f]
        CS[pp, 1] = -sin[f] if (pp % 64) < 32 else sin[f]
    return C, CS


PO_ = {}
_c = 0
for _name, _n in (("bada", DEPTH * 48), ("norm1", DEPTH * 8), ("norm2", DEPTH * 8),
                  ("pscale", DEPTH * 4), ("fnorm", 8)):
    PO_[_name] = _c
    _c += _n
NPAR = _c


class Buf:
    __slots__ = ("name", "w", "r", "excl")

    def __init__(self, name, excl=False):
        self.name = name
        self.w = None
        self.r = {}
        self.excl = excl


class Op:
    __slots__ = ("eng", "fn", "deps", "sig", "sem", "val", "is_dma", "dsem", "batch")


class DSem:
    def __init__(self):
        self.h = None
        self.count = 0


class Prog:
    ENGS = ("pe", "act", "dve", "pool", "sp")

    def __init__(self):
        self.q = {e: [] for e in self.ENGS}
        self.dsems = []
        self.nops = 0

    def dsem(self):
        s = DSem()
        self.dsems.append(s)
        return s

    def _add(self, eng, fn, reads, writes, is_dma, dsem, batch):
        o = Op()
        o.eng = eng
        o.fn = fn
        o.sig = False
        o.sem = None
        o.val = 0
        o.is_dma = is_dma
        o.dsem = dsem
        o.batch = batch
        deps = {}

        def add(d):
            if d is None:
                return
            if (not d.is_dma) and (not is_dma) and d.eng == "pe" and eng == "pe":
                return
            deps[id(d)] = d

        for b in reads:
            add(b.w)
            if b.excl:
                for k_, x in b.r.items():
                    if k_ != eng:
                        add(x)
        for b in writes:
            add(b.w)
            for x in b.r.values():
                add(x)
        o.deps = list(deps.values())
        for d in o.deps:
            d.sig = True
        if is_dma:
            o.sig = True
        key = ("dma", id(o)) if is_dma else eng
        for b in writes:
            b.w = o
            b.r = {}
        for b in reads:
            b.r[key] = o
        self.q[eng].append(o)
        self.nops += 1
        return o

    def op(self, eng, fn, reads=(), writes=()):
        return self._add(eng, fn, reads, writes, False, None, None)

    def dma(self, eng, fn, reads, writes, dsem, batch=None):
        o = self._add(eng, fn, reads, writes, True, dsem, batch)
        if batch is not None:
            batch.append(o)
        return o

    def emit(self, nc, stack):
        esems = {}
        for e in self.ENGS:
            n = sum(1 for o in self.q[e] if o.sig and not o.is_dma)
            esems[e] = [stack.enter_context(nc.semaphore(f"s_{e}_{i}")) for i in range(n // EPOCH + 1)]
            c = 0
            for o in self.q[e]:
                if o.sig and not o.is_dma:
                    o.sem = esems[e][c // EPOCH]
                    o.val = c % EPOCH + 1
                    c += 1
        for i, s in enumerate(self.dsems):
            s.h = stack.enter_context(nc.semaphore(f"s_dma_{i}"))
        done_batches = set()
        for e in self.ENGS:
            for o in self.q[e]:
                if not o.is_dma:
                    continue
                o.sem = o.dsem.h
                if o.batch is None:
                    o.dsem.count += 16
                    o.val = o.dsem.count
                elif id(o.batch) not in done_batches:
                    done_batches.add(id(o.batch))
                    fin = o.dsem.count + 16 * len(o.batch)
                    o.dsem.count = fin
                    for x in o.batch:
                        x.val = fin
        block = stack.enter_context(nc.Block())

        def run(eng_name):
            def body(eng):
                seen = {}
                for o in self.q[eng_name]:
                    for d in o.deps:
                        k = id(d.sem)
                        if seen.get(k, 0) >= d.val:
                            continue
                        seen[k] = d.val
                        eng.wait_ge(d.sem, d.val)
                    ins = o.fn(eng)
                    if o.sig:
                        ins.then_inc(o.sem, 16 if o.is_dma else 1)
            return body

        block.tensor(run("pe"))
        block.scalar(run("act"))
        block.vector(run("dve"))
        block.gpsimd(run("pool"))
        block.sync(run("sp"))


def build_program(n_layers=DEPTH, passes=((0, 0), (0, 1), (1, 0), (1, 1))):
    nc = bass.Bass("TRN2", target_bir_lowering=False)
    P = Prog()
    stack = contextlib.ExitStack()
    n_blocks_total = n_layers * NB_LAYER

    xT = nc.dram_tensor("xT", [2, 8, 128, SEQ], F32, kind="ExternalInput").ap()
    cT = nc.dram_tensor("cT", [128, 8, 2], F32, kind="ExternalInput").ap()
    wst = nc.dram_tensor("wst", [DEPTH * NB_LAYER, 128, BLK], F32, kind="ExternalInput").ap()
    wada = nc.dram_tensor("wada", [DEPTH * 12, 128, 8 * 512], F32, kind="ExternalInput").ap()
    par = nc.dram_tensor("par", [128, NPAR], F32, kind="ExternalInput").ap()
    cst = nc.dram_tensor("cst", [128, NCONST], F32, kind="ExternalInput").ap()
    csd = nc.dram_tensor("csd", [128, 2, SEQ], F32, kind="ExternalInput").ap()
    outT = nc.dram_tensor("outT", [2, 8, 128, SEQ], F32, kind="ExternalOutput").ap()

    def sb(name, shape, dt):
        return stack.enter_context(nc.sbuf_tensor(name, shape, dt))

    x_sb = sb("x_sb", [128, 8, TT], F32)
    h_sb = sb("h_sb", [128, 8, TT], BF16)
    act_sb = sb("act_sb", [128, NFF, TT], BF16)
    rg_sb = sb("rg_sb", [128, 8, TT], BF16)
    scr_sb = sb("scr_sb", [128, NSCR, SCRW], F32)
    pl_sb = sb("pl_sb", [128, 3, 16 + TT], F32)
    cs_sb = sb("cs_sb", [128, 2, TT], F32)
    cst_sb = sb("cst_sb", [128, NCONST], F32)
    par_sb = sb("par_sb", [128, NPAR], F32)
    mod_sb = sb("mod_sb", [128, DEPTH, 48, 2], F32)
    a_sb = sb("a_sb", [128, DEPTH, 2, 8, 2], F32)
    cact_sb = sb("cact_sb", [128, 8, 2], F32)
    cbf_sb = sb("cbf_sb", [128, 2, 128], BF16)
    stg_sb = sb("stg_sb", [128, NS, BLK], F32)
    wbf_sb = sb("wbf_sb", [128, NW, BLK], BF16)
    st32_sb = sb("st32_sb", [128, DEPTH, 4, 128], F32)
    stb_sb = sb("stb_sb", [128, DEPTH, 2, 4, 128], BF16)
    carry_sb = sb("carry_sb", [128, DEPTH, 4, 16], F32)
    ps_t = [stack.enter_context(nc.psum_tensor(f"ps{i}", [128, 512], F32)) for i in range(8)]

    X = [[Buf(f"x{j}{s}") for s in range(NSUB)] for j in range(8)]
    H = [[Buf(f"h{j}{s}") for s in range(NSUB)] for j in range(8)]
    A = [[Buf(f"a{i}{s}") for s in range(NSUB)] for i in range(NFF)]
    RG = [[Buf(f"rg{j}{s}") for s in range(NSUB)] for j in range(8)]
    SCR = [Buf(f"scr{i}") for i in range(NSCR)]
    PL = [Buf(f"pl{i}") for i in range(3)]
    CSB = Buf("cs")
    CSTB = Buf("cst")
    PARB = Buf("par")
    MODB = Buf("mod")
    CACTB = Buf("cact")
    CBFB = Buf("cbf")
    STG = [Buf(f"stg{i}") for i in range(NS)]
    WBF = [Buf(f"wbf{i}") for i in range(NW)]
    ST32 = [Buf(f"st32_{l}") for l in range(DEPTH)]
    STB = [[Buf(f"stb_{l}_{i}") for i in range(2)] for l in range(DEPTH)]
    CARRY = [[Buf(f"carry_{l}_{g}") for g in range(4)] for l in range(DEPTH)]
    PS = [Buf(f"psb{i}", excl=True) for i in range(8)]

    state = {"scr": 0, "ps": 0}

    def scr():
        i = state["scr"]
        state["scr"] = (i + 1) % NSCR
        return scr_sb[:, i, :], SCR[i]

    def psum():
        i = state["ps"]
        state["ps"] = (i + 1) % 8
        return ps_t[i][:], PS[i]

    def cview(name, n):
        return cst_sb[:, CO[name]:CO[name] + n]

    class WS:
        def __init__(self):
            self.next_dma = 0
            self.next_cast = 0
            self.maxb = -1
            self.sems = [P.dsem() for _ in range(NS)]
            self.order = []
            self.ncast = 0

        def set_order(self, order):
            self.order = order

        def _dma(self, b):
            if b >= len(self.order):
                return
            slot = b % NS
            src = wst[self.order[b]]
            P.dma("sp", lambda e, slot=slot, src=src: e.dma_start(out=stg_sb[:, slot, :], in_=src),
                  reads=[], writes=[STG[slot]], dsem=self.sems[slot])

        def _cast(self, b):
            while self.next_dma < b + NS:
                self._dma(self.next_dma)
                self.next_dma += 1
            ss, ws = b % NS, b % NW
            eng = ("act", "dve", "act", "dve", "pool")[self.ncast % 5]
            self.ncast += 1
            if eng == "dve":
                P.op("dve", lambda e, ss=ss, ws=ws: e.tensor_copy(out=wbf_sb[:, ws, :], in_=stg_sb[:, ss, :]),
                     reads=[STG[ss]], writes=[WBF[ws]])
            elif eng == "pool":
                P.op("pool", lambda e, ss=ss, ws=ws: e.tensor_copy(out=wbf_sb[:, ws, :], in_=stg_sb[:, ss, :]),
                     reads=[STG[ss]], writes=[WBF[ws]])
            else:
                P.op("act", lambda e, ss=ss, ws=ws: e.copy(out=wbf_sb[:, ws, :], in_=stg_sb[:, ss, :]),
                     reads=[STG[ss]], writes=[WBF[ws]])
            if self.next_dma == b + NS:
                self._dma(self.next_dma)
                self.next_dma += 1

        def touch(self, b):
            tgt = min(b + LOOKAHEAD, len(self.order) - 1)
            while self.next_cast <= tgt:
                self._cast(self.next_cast)
                self.next_cast += 1
            if b > self.maxb:
                self.maxb = b
            assert b > self.maxb - WINDOW, (b, self.maxb)

        def tile(self, base_blk, t, n=1):
            b = base_blk + t // BLK_TILES
            assert (t % BLK_TILES) + n <= BLK_TILES
            self.touch(b)
            ws = b % NW
            c0 = (t % BLK_TILES) * 128
            return wbf_sb[:, ws, c0:c0 + 128 * n], WBF[ws]

    ws = WS()
    order = []
    for (sq, hf) in passes:
        for l in range(n_layers):
            order.extend(range(l * NB_LAYER, (l + 1) * NB_LAYER))
    ws.set_order(order)

    ld_sem = P.dsem()
    ldb = []
    P.dma("sp", lambda e: e.dma_start(out=cst_sb[:], in_=cst[:, :]), [], [CSTB], ld_sem, ldb)
    P.dma("sp", lambda e: e.dma_start(out=par_sb[:], in_=par[:, :]), [], [PARB], ld_sem, ldb)
    P.dma("sp", lambda e: e.dma_start(out=cact_sb[:], in_=cT[:, :, :]), [], [CACTB], ld_sem, ldb)
    P.op("dve", lambda e: e.tensor_copy(out=cbf_sb[:, 0, :], in_=cview("ident", 128)), [CSTB], [CBFB])
    P.op("dve", lambda e: e.tensor_copy(out=cbf_sb[:, 1, :], in_=cview("pswap", 128)), [CBFB, CSTB], [CBFB])
    P.op("act", lambda e: e.activation(out=cact_sb[:], in_=cact_sb[:], func=AF.Silu), [CACTB], [CACTB])
    ident = cbf_sb[:, 0, :]
    pswap = cbf_sb[:, 1, :]
    onesd = cview("onesd", 128)
    onesh = cview("onesh", 128)

    wa_f32 = act_sb[:].rearrange("p a b -> p (a b)").bitcast(F32)
    WA = [Buf("wa0"), Buf("wa1")]
    wa_sems = [P.dsem(), P.dsem()]
    allA = [A[i][s] for i in range(NFF) for s in range(NSUB)]
    for l in range(n_layers):
        mps, mpsb = psum()
        for pc in range(12):
            slot = (l * 12 + pc) % 2
            src = wada[l * 12 + pc]
            P.dma("sp", lambda e, slot=slot, src=src: e.dma_start(out=wa_f32[:, slot * 4096:(slot + 1) * 4096], in_=src),
                  [], [WA[slot]], wa_sems[slot])
            for ct in range(4):
                t = pc * 4 + ct
                for kk in range(8):
                    P.op("pe", lambda e, slot=slot, ct=ct, kk=kk, t=t, mps=mps: e.matmul(
                        mps[:, 2 * t:2 * t + 2],
                        wa_f32[:, slot * 4096 + kk * 512 + ct * 128: slot * 4096 + kk * 512 + ct * 128 + 128],
                        cact_sb[:, kk, :], start=(kk == 0), stop=(kk == 7)),
                        [WA[slot], CACTB], [mpsb])
        bada = par_sb[:, PO_["bada"] + l * 48: PO_["bada"] + (l + 1) * 48]
        P.op("dve", lambda e, l=l, mps=mps, bada=bada: e.tensor_tensor(
            out=mod_sb[:, l, :, :], in0=mps[:, 0:96].rearrange("p (t b) -> p t b", b=2),
            in1=bada.unsqueeze(2).broadcast_to([128, 48, 2]), op=ALU.add), [mpsb, PARB], [MODB])
        for k, (t0, nm) in enumerate(((8, "norm1"), (32, "norm2"))):
            nv = par_sb[:, PO_[nm] + l * 8: PO_[nm] + (l + 1) * 8]
            P.op("dve", lambda e, l=l, k=k, t0=t0, nv=nv: e.scalar_tensor_tensor(
                out=a_sb[:, l, k, :, :], in0=mod_sb[:, l, t0:t0 + 8, :], scalar=1.0,
                in1=nv.unsqueeze(2).broadcast_to([128, 8, 2]), op0=ALU.add, op1=ALU.mult), [MODB, PARB], [MODB])
    P.op("pool", lambda e: e.memset(act_sb[:, 0, 0:16], 0.0), [], allA + WA)

    def modv(l, t, b):
        return mod_sb[:, l, t, b:b + 1]

    def rmsnorm_to_h(l, bsel, which):
        tB = 0 if which == 0 else 24
        for s in range(NSUB):
            sl = slice(s * 512, (s + 1) * 512)
            stp, stb_ = psum()
            for j in range(8):
                sq, sqb = scr()
                P.op("act", lambda e, j=j, sq=sq, sl=sl: e.activation(out=sq[:, 0:512], in_=x_sb[:, j, sl], func=AF.Square),
                     [X[j][s]], [sqb])
                P.op("pe", lambda e, j=j, sq=sq, stp=stp: e.matmul(stp, onesd, sq[:, 0:512], start=(j == 0), stop=(j == 7)),
                     [sqb, CSTB], [stb_])
            rs, rsb = scr()
            P.op("act", lambda e, rs=rs, stp=stp: e.activation(out=rs[:, 0:512], in_=stp, func=AF.Sqrt, bias=EPS, scale=1.0),
                 [stb_], [rsb])
            P.op("dve", lambda e, rs=rs: e.reciprocal(out=rs[:, 0:512], in_=rs[:, 0:512]), [rsb], [rsb])
            for j in range(8):
                tm, tmb = scr()
                P.op("dve", lambda e, j=j, tm=tm, rs=rs, sl=sl: e.tensor_tensor(
                    out=tm[:, 0:512], in0=x_sb[:, j, sl], in1=rs[:, 0:512], op=ALU.mult), [X[j][s], rsb], [tmb])
                P.op("act", lambda e, j=j, tm=tm, sl=sl: e.activation(
                    out=h_sb[:, j, sl], in_=tm[:, 0:512], func=AF.Identity,
                    bias=modv(l, tB + j, bsel), scale=a_sb[:, l, which, j, bsel:bsel + 1]),
                    [tmb, MODB], [H[j][s]])

    def proj_fm(base, t0, s, nk=8, rhs_of=None):
        ps, psb = psum()
        sl = slice(s * 512, (s + 1) * 512)
        for kk in range(nk):
            w, wb = ws.tile(base, t0 + kk)
            r, rb = rhs_of(kk, s, sl)
            P.op("pe", lambda e, ps=ps, w=w, r=r, kk=kk: e.matmul(ps, w, r, start=(kk == 0), stop=(kk == nk - 1)),
                 [wb, rb], [psb])
        return ps, psb

    def rhs_h(kk, s, sl):
        return h_sb[:, kk, sl], H[kk][s]

    def rhs_rg(kk, s, sl):
        return rg_sb[:, kk, sl], RG[kk][s]

    def rhs_a(off):
        def f(kk, s, sl):
            return act_sb[:, off + kk, sl], A[off + kk][s]
        return f

    def layer(l, bsel, hf, base):
        pos0 = hf * TT
        rmsnorm_to_h(l, bsel, 0)

        if DEBUG_STOP == 1:
            return
        def rot_stage2(part, j, s, sl, rawbf, rawb, t1, t1b):
            ps2, ps2b = psum()
            P.op("pe", lambda e: e.matmul(ps2, pswap, rawbf, start=True, stop=True), [rawb, CBFB], [ps2b])
            t2, t2b = scr()
            P.op("dve", lambda e: e.tensor_tensor(out=t2[:, 0:512], in0=ps2, in1=cs_sb[:, 1, sl], op=ALU.mult),
                 [ps2b, CSB], [t2b])
            if part == 1:
                P.op("pool", lambda e: e.tensor_tensor(
                    out=act_sb[:, 4 + j, sl], in0=t1[:, 0:512], in1=t2[:, 0:512], op=ALU.add),
                    [t1b, t2b], [A[4 + j][s]])
            else:
                P.op("pool", lambda e: e.tensor_tensor(
                    out=t1[:, 0:512], in0=t1[:, 0:512], in1=t2[:, 0:512], op=ALU.add), [t1b, t2b], [t1b])
                qd = cst_sb[:, CO["qdec"] + j * 128: CO["qdec"] + (j + 1) * 128]
                P.op("pool", lambda e: e.tensor_tensor(
                    out=act_sb[:, j, sl].rearrange("p (a b) -> p a b", a=4),
                    in0=t1[:, 0:512].rearrange("p (a b) -> p a b", a=4),
                    in1=qd.unsqueeze(1).broadcast_to([128, 4, 128]), op=ALU.mult),
                    [t1b, CSTB], [A[j][s]])

        pending = None
        for part in range(2):
            for j in range(4):
                for s in range(NSUB):
                    sl = slice(s * 512, (s + 1) * 512)
                    ps, psb = proj_fm(base, OFF["q" if part == 0 else "k"] + j * 8, s, 8, rhs_h)
                    raw, rawb = scr()
                    rawbf = raw.bitcast(BF16)[:, 0:512]
                    P.op("act", lambda e, rawbf=rawbf, ps=ps: e.copy(out=rawbf, in_=ps), [psb], [rawb])
                    t1, t1b = scr()
                    P.op("dve", lambda e, t1=t1, ps=ps, sl=sl: e.tensor_tensor(
                        out=t1[:, 0:512], in0=ps, in1=cs_sb[:, 0, sl], op=ALU.mult), [psb, CSB], [t1b])
                    if pending is not None:
                        rot_stage2(*pending)
                    pending = (part, j, s, sl, rawbf, rawb, t1, t1b)
        rot_stage2(*pending)

        kdec = cview("kdec", 8)

        def k_transpose(n):
            s, c0 = n // 4, (n % 4) * 128
            ps, psb = psum()
            psbf = ps.bitcast(BF16)
            for j in range(4):
                P.op("pe", lambda e, j=j: e.transpose(
                    psbf[:, j * 128:(j + 1) * 128], act_sb[:, 4 + j, s * 512 + c0: s * 512 + c0 + 128], ident),
                    [A[4 + j][s], CBFB], [psb])
            P.op("dve", lambda e: e.tensor_tensor(
                out=act_sb[:, 16 + n // 2, (n % 2) * 512:(n % 2 + 1) * 512].rearrange("p (h a) -> p h a", h=8),
                in0=psbf[:, 0:512].rearrange("p (h a) -> p h a", h=8),
                in1=kdec.unsqueeze(2).broadcast_to([128, 8, 64]), op=ALU.mult),
                [psb, CSTB], [A[16 + n // 2][n % 2]])

        gi = 0
        for cg in range(2):
            for n in range(NCH):
                s, c0 = n // 4, (n % 4) * 128
                ps, psb = psum()
                for kk in range(8):
                    w, wb = ws.tile(base, OFF["v"] + (cg * 8 + kk) * 4, 4)
                    P.op("pe", lambda e, ps=ps, w=w, kk=kk, s=s, c0=c0: e.matmul(
                        ps, h_sb[:, kk, s * 512 + c0: s * 512 + c0 + 128], w, start=(kk == 0), stop=(kk == 7)),
                        [wb, H[kk][s]], [psb])
                P.op("dve", lambda e, ps=ps, n=n, cg=cg: e.tensor_copy(out=act_sb[:, 8 + n, cg * 512:(cg + 1) * 512], in_=ps),
                     [psb], [A[8 + n][cg]])
                if gi % 2 == 1:
                    k_transpose(gi // 2)
                gi += 1

        if hf == 0:
            P.op("pool", lambda e: e.memset(st32_sb[:, l, :, :], 0.0), [], [ST32[l]])
            P.op("pool", lambda e: e.memset(stb_sb[:, l, 0, :, :], 0.0), [], [STB[l][0]])

        mask = cview("mask", 1024)
        cdec = cview("cdec", 4)
        mask4 = mask.rearrange("p (j q c) -> p j q c", j=4, q=2)
        rg4 = rg_sb[:].rearrange("p (j q) t -> p j q t", q=2)
        for n in range(NCH):
            s, c0 = n // 4, (n % 4) * 128
            tsl = slice(s * 512 + c0, s * 512 + c0 + 128)
            vbufs = [A[8 + n][0], A[8 + n][1]]
            cur, nxt = n % 2, (n + 1) % 2
            sc_ps = [psum(), psum()]
            for h in range(8):
                j, par = h // 2, h % 2
                p0 = par * 64
                bank, bankb = sc_ps[par]
                P.op("pe", lambda e, bank=bank, j=j, p0=p0, tsl=tsl: e.matmul(
                    bank[:, j * 128:(j + 1) * 128], act_sb[p0:p0 + 64, 4 + j, tsl], act_sb[p0:p0 + 64, j, tsl],
                    start=True, stop=True), [A[4 + j][s], A[j][s]], [bankb])
            pkv, pkvb = psum()
            kdb = A[16 + n // 2][n % 2]
            for h in range(8):
                P.op("pe", lambda e, pkv=pkv, h=h, n=n: e.matmul(
                    pkv[(h % 2) * 64:(h % 2) * 64 + 64, (h // 2) * 128:(h // 2 + 1) * 128],
                    act_sb[:, 16 + n // 2, (n % 2) * 512 + h * 64:(n % 2) * 512 + (h + 1) * 64],
                    act_sb[:, 8 + n, h * 128:(h + 1) * 128], start=True, stop=True),
                    [kdb, vbufs[h // 4]], [pkvb])
            stts = []
            for par in range(2):
                bank, bankb = sc_ps[par]
                stt, sttb = scr()
                sttbf = stt.bitcast(BF16)[:, 0:512]
                P.op("dve", lambda e, sttbf=sttbf, bank=bank, par=par: e.tensor_tensor(
                    out=sttbf.rearrange("p (j c) -> p j c", j=4), in0=bank.rearrange("p (j c) -> p j c", j=4),
                    in1=mask4[:, :, par, :], op=ALU.mult), [bankb, CSTB], [sttb])
                stts.append((sttbf, sttb))
            tmp, tmpb = scr()
            P.op("pool", lambda e, tmp=tmp: e.tensor_tensor(
                out=tmp[:, 0:512].rearrange("p (a b) -> p a b", a=4), in0=st32_sb[:, l, :, :],
                in1=cdec.unsqueeze(2).broadcast_to([128, 4, 128]), op=ALU.mult), [ST32[l], CSTB], [tmpb])
            P.op("dve", lambda e, tmp=tmp, pkv=pkv: e.tensor_tensor(
                out=st32_sb[:, l, :, :], in0=tmp[:, 0:512].rearrange("p (a b) -> p a b", a=4),
                in1=pkv.rearrange("p (a b) -> p a b", a=4), op=ALU.add), [tmpb, pkvb], [ST32[l]])
            P.op("act", lambda e, nxt=nxt: e.copy(out=stb_sb[:, l, nxt, :, :], in_=st32_sb[:, l, :, :]),
                 [ST32[l]], [STB[l][nxt]])
            o_ps = [psum(), psum()]
            for h in range(8):
                j, par = h // 2, h % 2
                p0 = par * 64
                bank, bankb = o_ps[par]
                sttbf, sttb = stts[par]
                P.op("pe", lambda e, bank=bank, j=j, h=h, n=n, sttbf=sttbf: e.matmul(
                    bank[:, j * 128:(j + 1) * 128], act_sb[:, 8 + n, h * 128:(h + 1) * 128],
                    sttbf[:, j * 128:(j + 1) * 128], start=True, stop=False),
                    [vbufs[h // 4], sttb], [bankb])
                P.op("pe", lambda e, bank=bank, j=j, p0=p0, tsl=tsl, cur=cur: e.matmul(
                    bank[:, j * 128:(j + 1) * 128], stb_sb[p0:p0 + 64, l, cur, j, :],
                    act_sb[p0:p0 + 64, j, tsl], start=False, stop=True),
                    [STB[l][cur], A[j][s]], [bankb])
            for par in range(2):
                bank, bankb = o_ps[par]
                osq, osqb = scr()
                P.op("act", lambda e, osq=osq, bank=bank: e.activation(out=osq[:, 0:512], in_=bank, func=AF.Square),
                     [bankb], [osqb])
                psn, psnb = psum()
                P.op("pe", lambda e, psn=psn, osq=osq: e.matmul(psn, onesh, osq[:, 0:512], start=True, stop=True),
                     [osqb, CSTB], [psnb])
                hr, hrb = scr()
                P.op("act", lambda e, hr=hr, psn=psn: e.activation(out=hr[:, 0:512], in_=psn, func=AF.Sqrt, bias=EPS, scale=1.0),
                     [psnb], [hrb])
                P.op("dve", lambda e, hr=hr: e.reciprocal(out=hr[:, 0:512], in_=hr[:, 0:512]), [hrb], [hrb])
                P.op("dve", lambda e, hr=hr, bank=bank, par=par, tsl=tsl: e.tensor_tensor(
                    out=rg4[:, :, par, tsl], in0=bank.rearrange("p (a b) -> p a b", a=4),
                    in1=hr[:, 0:512].rearrange("p (a b) -> p a b", a=4), op=ALU.mult),
                    [hrb, bankb], [RG[2 * i + par][s] for i in range(4)])

        if DEBUG_STOP == 5:
            return
        for h in range(8):
            for s in range(NSUB):
                sl = slice(s * 512, (s + 1) * 512)
                ps, psb = proj_fm(base, OFF["g"] + h * 8, s, 8, rhs_h)
                sg, sgb = scr()
                sgbf = sg.bitcast(BF16)[:, 0:512]
                P.op("act", lambda e, sgbf=sgbf, ps=ps: e.activation(out=sgbf, in_=ps, func=AF.Silu), [psb], [sgb])
                P.op("pool", lambda e, h=h, sl=sl, sgbf=sgbf: e.tensor_tensor(
                    out=rg_sb[:, h, sl], in0=rg_sb[:, h, sl], in1=sgbf, op=ALU.mult), [sgb, RG[h][s]], [RG[h][s]])

        if DEBUG_STOP == 6:
            return
        invc = cview("invcnt", 64)
        for g in range(4):
            w = 2 << g
            for s in range(NSUB):
                ps, psb = proj_fm(base, OFF["p"] + g * 8, s, 8, rhs_h)
                P.op("act", lambda e, ps=ps, s=s: e.copy(out=pl_sb[:, 0, 16 + s * 512:16 + (s + 1) * 512], in_=ps),
                     [psb], [PL[0]])
            if hf == 0:
                P.op("pool", lambda e: e.memset(pl_sb[:, 0, 0:16], 0.0), [], [PL[0]])
            else:
                P.op("pool", lambda e, g=g: e.tensor_copy(out=pl_sb[:, 0, 0:16], in_=carry_sb[:, l, g, :]),
                     [CARRY[l][g]], [PL[0]])
            P.op("pool", lambda e, g=g: e.tensor_copy(out=carry_sb[:, l, g, :], in_=pl_sb[:, 0, TT:TT + 16]),
                 [PL[0]], [CARRY[l][g]])
            cur, curb = 0, PL[0]
            sh, lo = 1, 0
            while sh < w:
                nxt = 1 if cur != 1 else 2
                lo2 = lo + sh
                P.op("pool", lambda e, cur=cur, nxt=nxt, sh=sh, lo2=lo2: e.tensor_tensor(
                    out=pl_sb[:, nxt, lo2:16 + TT], in0=pl_sb[:, cur, lo2:16 + TT],
                    in1=pl_sb[:, cur, lo2 - sh:16 + TT - sh], op=ALU.add), [PL[cur]], [PL[nxt]])
                cur, lo, sh = nxt, lo2, sh * 2
            P.op("dve", lambda e, cur=cur, g=g, w=w: e.scalar_tensor_tensor(
                out=act_sb[:, 8 + g, :], in0=pl_sb[:, cur, 16:16 + TT], scalar=1.0 / w,
                in1=pl_sb[:, 0, 16:16 + TT], op0=ALU.mult, op1=ALU.subtract), [PL[cur], PL[0]], [A[8 + g][0], A[8 + g][1]])
            if hf == 0:
                tm, tmb = scr()
                P.op("dve", lambda e, tm=tm, cur=cur, g=g: e.tensor_tensor(
                    out=tm[:, 0:16], in0=pl_sb[:, cur, 16:32], in1=invc[:, g * 16:(g + 1) * 16], op=ALU.mult),
                    [PL[cur], CSTB], [tmb])
                P.op("dve", lambda e, tm=tm, g=g: e.tensor_tensor(
                    out=act_sb[:, 8 + g, 0:16], in0=tm[:, 0:16], in1=pl_sb[:, 0, 16:32], op=ALU.subtract),
                    [tmb, PL[0], A[8 + g][0]], [A[8 + g][0]])
        for g in range(4):
            for s in range(NSUB):
                sl = slice(s * 512, (s + 1) * 512)
                ps, psb = psum()
                wt, wtb = ws.tile(base, OFF["grp"] + g)
                P.op("pe", lambda e, ps=ps, wt=wt, g=g, sl=sl: e.matmul(ps, wt, act_sb[:, 8 + g, sl], start=True, stop=True),
                     [wtb, A[8 + g][s]], [psb])
                psc = par_sb[:, PO_["pscale"] + l * 4 + g: PO_["pscale"] + l * 4 + g + 1]
                P.op("act", lambda e, ps=ps, g=g, sl=sl, psc=psc: e.activation(
                    out=act_sb[:, 12 + g, sl], in_=ps, func=AF.Identity, bias=0.0, scale=psc), [psb, PARB], [A[12 + g][s]])

        if DEBUG_STOP == 7:
            return
        for j in range(8):
            m0 = OFF["mrg"] + j * 28
            for s in range(NSUB):
                sl = slice(s * 512, (s + 1) * 512)
                prd, prdb = proj_fm(base, m0, s, 8, rhs_rg)
                ppd, ppdb = proj_fm(base, m0 + 8, s, 4, rhs_a(12))
                par_, parb_ = proj_fm(base, m0 + 12, s, 8, rhs_h)
                pap, papb = proj_fm(base, m0 + 20, s, 8, rhs_h)
                sr, srb = scr()
                P.op("act", lambda e, sr=sr, par_=par_: e.activation(out=sr[:, 0:512], in_=par_, func=AF.Sigmoid), [parb_], [srb])
                sp_, spb = scr()
                P.op("act", lambda e, sp_=sp_, pap=pap: e.activation(out=sp_[:, 0:512], in_=pap, func=AF.Sigmoid), [papb], [spb])
                P.op("dve", lambda e, sr=sr, prd=prd: e.tensor_tensor(out=sr[:, 0:512], in0=prd, in1=sr[:, 0:512], op=ALU.mult),
                     [prdb, srb], [srb])
                P.op("dve", lambda e, sp_=sp_, ppd=ppd: e.tensor_tensor(out=sp_[:, 0:512], in0=ppd, in1=sp_[:, 0:512], op=ALU.mult),
                     [ppdb, spb], [spb])
                P.op("pool", lambda e, j=j, sl=sl, sr=sr, sp_=sp_: e.tensor_tensor(
                    out=act_sb[:, j, sl], in0=sr[:, 0:512], in1=sp_[:, 0:512], op=ALU.add), [srb, spb], [A[j][s]])

        if DEBUG_STOP == 8:
            return
        for j in range(8):
            for s in range(NSUB):
                sl = slice(s * 512, (s + 1) * 512)
                ps, psb = proj_fm(base, OFF["out"] + j * 8, s, 8, rhs_a(0))
                P.op("dve", lambda e, ps=ps, j=j, sl=sl: e.scalar_tensor_tensor(
                    out=x_sb[:, j, sl], in0=ps, scalar=modv(l, 16 + j, bsel), in1=x_sb[:, j, sl],
                    op0=ALU.mult, op1=ALU.add), [psb, MODB, X[j][s]], [X[j][s]])

        if DEBUG_STOP == 9:
            return
        rmsnorm_to_h(l, bsel, 1)
        for i in range(NFF):
            for s in range(NSUB):
                sl = slice(s * 512, (s + 1) * 512)
                pg, pgb = proj_fm(base, OFF["ffi"] + i * 16, s, 8, rhs_h)
                pu, pub = proj_fm(base, OFF["ffi"] + i * 16 + 8, s, 8, rhs_h)
                sg, sgb = scr()
                P.op("act", lambda e, sg=sg, pg=pg: e.activation(out=sg[:, 0:512], in_=pg, func=AF.Silu), [pgb], [sgb])
                P.op("dve", lambda e, sg=sg, pu=pu, i=i, sl=sl: e.tensor_tensor(
                    out=act_sb[:, i, sl], in0=pu, in1=sg[:, 0:512], op=ALU.mult), [pub, sgb], [A[i][s]])
        for j in range(8):
            for s in range(NSUB):
                sl = slice(s * 512, (s + 1) * 512)
                ps, psb = proj_fm(base, OFF["ffo"] + j * NFF, s, NFF, rhs_a(0))
                P.op("dve", lambda e, ps=ps, j=j, sl=sl: e.scalar_tensor_tensor(
                    out=x_sb[:, j, sl], in0=ps, scalar=modv(l, 40 + j, bsel), in1=x_sb[:, j, sl],
                    op0=ALU.mult, op1=ALU.add), [psb, MODB, X[j][s]], [X[j][s]])

    xl_sem = P.dsem()
    st_sem = P.dsem()
    cs_sem = P.dsem()
    store_ops = []
    blk = 0
    for (sq, hf) in passes:
        pos0 = hf * TT
        lb = []
        for j in range(8):
            for s in range(NSUB):
                P.dma("act", lambda e, j=j, s=s, sq=sq, pos0=pos0: e.dma_start(
                    out=x_sb[:, j, s * 512:(s + 1) * 512], in_=xT[sq, j, :, pos0 + s * 512: pos0 + (s + 1) * 512]),
                    [], [X[j][s]], xl_sem, lb)
        P.dma("act", lambda e, pos0=pos0: e.dma_start(out=cs_sb[:], in_=csd[:, :, pos0:pos0 + TT]), [], [CSB], cs_sem)
        for l in range(n_layers):
            layer(l, sq, hf, blk)
            blk += NB_LAYER
        fn = par_sb[:, PO_["fnorm"]: PO_["fnorm"] + 8]
        sbatch = []
        for s in range(NSUB):
            sl = slice(s * 512, (s + 1) * 512)
            stp, stb_ = psum()
            for j in range(8):
                sq_, sqb = scr()
                P.op("act", lambda e, j=j, sq_=sq_, sl=sl: e.activation(out=sq_[:, 0:512], in_=x_sb[:, j, sl], func=AF.Square),
                     [X[j][s]], [sqb])
                P.op("pe", lambda e, j=j, sq_=sq_, stp=stp: e.matmul(stp, onesd, sq_[:, 0:512], start=(j == 0), stop=(j == 7)),
                     [sqb, CSTB], [stb_])
            rs, rsb = scr()
            P.op("act", lambda e, rs=rs, stp=stp: e.activation(out=rs[:, 0:512], in_=stp, func=AF.Sqrt, bias=EPS, scale=1.0),
                 [stb_], [rsb])
            P.op("dve", lambda e, rs=rs: e.reciprocal(out=rs[:, 0:512], in_=rs[:, 0:512]), [rsb], [rsb])
            for j in range(8):
                P.op("dve", lambda e, j=j, rs=rs, sl=sl: e.scalar_tensor_tensor(
                    out=x_sb[:, j, sl], in0=x_sb[:, j, sl], scalar=fn[:, j:j + 1], in1=rs[:, 0:512],
                    op0=ALU.mult, op1=ALU.mult), [X[j][s], rsb, PARB], [X[j][s]])
                o = P.dma("act", lambda e, j=j, s=s, sq=sq, pos0=pos0, sl=sl: e.dma_start(
                    out=outT[sq, j, :, pos0 + s * 512: pos0 + (s + 1) * 512], in_=x_sb[:, j, sl]),
                    [X[j][s]], [], st_sem, sbatch)
                store_ops.append(o)
    fin = P.op("act", lambda e: e.activation(out=scr_sb[:, 0, 0:8], in_=scr_sb[:, 0, 0:8], func=AF.Copy), [], [SCR[0]])
    dd = {id(o): o for o in fin.deps}
    for o in store_ops:
        dd[id(o)] = o
    fin.deps = list(dd.values())

    P.emit(nc, stack)
    return nc, stack, P


def prep_shared(w_ada, b_ada, norm1, w_in, w_ret_o, w_pool_grp, pool_scale, w_pool_o,
                w_out, norm2, w_ffn_in, w_ffn_out, final_norm):
    f = lambda a: np.ascontiguousarray(np.asarray(a, dtype=np.float32))
    wst = np.concatenate([pack_layer(f(w_in[l]), f(w_ret_o[l]), f(w_pool_grp[l]), f(w_pool_o[l]), f(w_out[l]),
                                     f(w_ffn_in[l]), f(w_ffn_out[l])) for l in range(DEPTH)], axis=0)
    wa = f(w_ada).reshape(DEPTH, 8, 128, 12, 512).transpose(0, 3, 2, 1, 4).reshape(DEPTH * 12, 128, 8 * 512)
    par = np.zeros((128, NPAR), np.float32)
    par[:, PO_["bada"]:PO_["bada"] + DEPTH * 48] = f(b_ada).reshape(DEPTH, 48, 128).transpose(2, 0, 1).reshape(128, -1)
    par[:, PO_["norm1"]:PO_["norm1"] + DEPTH * 8] = f(norm1).reshape(DEPTH, 8, 128).transpose(2, 0, 1).reshape(128, -1)
    par[:, PO_["norm2"]:PO_["norm2"] + DEPTH * 8] = f(norm2).reshape(DEPTH, 8, 128).transpose(2, 0, 1).reshape(128, -1)
    par[:, PO_["pscale"]:PO_["pscale"] + DEPTH * 4] = f(pool_scale).reshape(DEPTH, 4, 128).transpose(2, 0, 1).reshape(128, -1)
    par[:, PO_["fnorm"]:PO_["fnorm"] + 8] = f(final_norm).reshape(8, 128).T
    cst, csd = make_consts()
    return {"wst": np.ascontiguousarray(wst), "wada": np.ascontiguousarray(wa), "par": par, "cst": cst, "csd": csd}


def prep_core(x, c, core):
    xs = np.asarray(x[2 * core:2 * core + 2], dtype=np.float32)
    xT = np.ascontiguousarray(xs.transpose(0, 2, 1).reshape(2, 8, 128, SEQ))
    cs = np.asarray(c[2 * core:2 * core + 2], dtype=np.float32)
    cT = np.ascontiguousarray(cs.reshape(2, 8, 128).transpose(2, 1, 0))
    return {"xT": xT, "cT": cT}


_CACHE = {}


def kernel(x, c, w_ada, b_ada, norm1, w_in, w_ret_o, w_pool_grp, pool_scale, w_pool_o,
           w_out, norm2, w_ffn_in, w_ffn_out, final_norm):
    shared = prep_shared(w_ada, b_ada, norm1, w_in, w_ret_o, w_pool_grp, pool_scale, w_pool_o,
                         w_out, norm2, w_ffn_in, w_ffn_out, final_norm)
    nc, stack, P = build_program()
    in_maps = []
    for core in range(NCORES):
        m = dict(shared)
        m.update(prep_core(x, c, core))
        in_maps.append(m)
    res = run_bass_kernel_spmd(nc, in_maps, core_ids=list(range(NCORES)))
    out = np.empty((BATCH, SEQ, D), np.float32)
    for core in range(NCORES):
        o = np.asarray(res.results[core]["outT"]).reshape(2, D, SEQ)
        out[2 * core:2 * core + 2] = o.transpose(0, 2, 1)
    return out
```

```python
import contextlib
import numpy as np
import concourse.bass as bass
import concourse.mybir as mybir
from concourse.bass_utils import run_bass_kernel_spmd

F32 = mybir.dt.float32
BF16 = mybir.dt.bfloat16
AF = mybir.ActivationFunctionType
ALU = mybir.AluOpType

D = 1024
SEQ = 2048
BATCH = 16
DEPTH = 4
NCORES = 8
TT = 1024
NSUB = 2
NCH = 8
DFF = 2816
NFF = 22
EPS = 1e-6
BLK_TILES = 8
BLK = BLK_TILES * 128
NS = 3
NW = 8
LOOKAHEAD = 3
WINDOW = NW - LOOKAHEAD
NSCR = 11
SCRW = 528
EPOCH = 12000
DEBUG_STOP = 0
DEBUG_VAR = 0

OFF = {}
_o = 0
for _name, _n in (("q", 32), ("k", 32), ("v", 64), ("p", 32), ("g", 64), ("grp", 4),
                  ("mrg", 8 * 28), ("out", 64), ("ffi", NFF * 16), ("ffo", 8 * NFF)):
    OFF[_name] = _o
    _o += _n
NT_LAYER = _o
NB_LAYER = -(-NT_LAYER // BLK_TILES)
assert OFF["v"] % 4 == 0


def _tiles_of(W):
    K, N = W.shape
    return W.reshape(K // 128, 128, N // 128, 128).transpose(2, 0, 1, 3)


def pack_layer(w_in, w_ret_o, w_pool_grp, w_pool_o, w_out, w_ffn_in, w_ffn_out):
    T = np.zeros((NB_LAYER * BLK_TILES, 128, 128), np.float32)
    tin = _tiles_of(w_in)
    T[OFF["q"]:OFF["q"] + 32] = tin[0:4].reshape(32, 128, 128)
    T[OFF["k"]:OFF["k"] + 32] = tin[4:8].reshape(32, 128, 128)
    tv = tin[8:16].reshape(2, 4, 8, 128, 128).transpose(0, 2, 1, 3, 4)
    T[OFF["v"]:OFF["v"] + 64] = tv.reshape(64, 128, 128)
    T[OFF["g"]:OFF["g"] + 64] = tin[16:24].reshape(64, 128, 128)
    T[OFF["p"]:OFF["p"] + 32] = tin[24:28].reshape(32, 128, 128)
    T[OFF["grp"]:OFF["grp"] + 4] = w_pool_grp
    tro = _tiles_of(w_ret_o)
    tpo = _tiles_of(w_pool_o)
    m = OFF["mrg"]
    for j in range(8):
        T[m:m + 8] = tro[j]
        T[m + 8:m + 12] = tpo[j]
        T[m + 12:m + 20] = tin[28 + j]
        T[m + 20:m + 28] = tin[36 + j]
        m += 28
    T[OFF["out"]:OFF["out"] + 64] = _tiles_of(w_out).reshape(64, 128, 128)
    tfi = _tiles_of(w_ffn_in)
    f = OFF["ffi"]
    for i in range(NFF):
        T[f:f + 8] = tfi[i]
        T[f + 8:f + 16] = tfi[NFF + i]
        f += 16
    T[OFF["ffo"]:OFF["ffo"] + 8 * NFF] = _tiles_of(w_ffn_out).reshape(8 * NFF, 128, 128)
    return T.reshape(NB_LAYER, BLK_TILES, 128, 128).transpose(0, 2, 1, 3).reshape(NB_LAYER, 128, BLK)


CO = {}
_c = 0
for _name, _n in (("mask", 1024), ("qdec", 512), ("kdec", 8), ("cdec", 4), ("invcnt", 64),
                  ("ident", 128), ("pswap", 128), ("onesd", 128), ("onesh", 128)):
    CO[_name] = _c
    _c += _n
NCONST = _c


def make_consts():
    C = np.zeros((128, NCONST), np.float64)
    gam = 1.0 - 2.0 ** (-5.0 - np.arange(8))
    p = np.arange(128)
    idx = np.arange(128)
    msk = np.zeros((128, 8, 128))
    for h in range(8):
        msk[:, h, :] = (idx[None, :] >= idx[:, None]) * (gam[h] ** (-(idx[:, None] + 1.0)))
    C[:, CO["mask"]:CO["mask"] + 1024] = msk.reshape(128, 1024)
    qd = np.zeros((128, 4, 128))
    for j in range(4):
        for half in range(2):
            qd[half * 64:(half + 1) * 64, j, :] = (64 ** -0.5) * gam[2 * j + half] ** (idx[None, :] + 1.0)
    C[:, CO["qdec"]:CO["qdec"] + 512] = qd.reshape(128, 512)
    for h in range(8):
        C[:, CO["kdec"] + h] = gam[h] ** (127.0 - p)
    for j in range(4):
        C[0:64, CO["cdec"] + j] = gam[2 * j] ** 128.0
        C[64:128, CO["cdec"] + j] = gam[2 * j + 1] ** 128.0
    for g, w in enumerate((2, 4, 8, 16)):
        C[:, CO["invcnt"] + g * 16:CO["invcnt"] + (g + 1) * 16] = 1.0 / np.minimum(np.arange(16) + 1.0, float(w))
    C[:, CO["ident"]:CO["ident"] + 128] = np.eye(128)
    C[p, CO["pswap"] + (p ^ 32)] = 1.0
    C[:, CO["onesd"]:CO["onesd"] + 128] = 1.0 / 1024.0
    C[:, CO["onesh"]:CO["onesh"] + 128] = 1.0 / 128.0
    C = C.astype(np.float32)
    half = 32
    inv_freq = (10000.0 ** (-(np.arange(half, dtype=np.float32) / half))).astype(np.float32)
    ang = (np.arange(SEQ, dtype=np.float32)[None, :] * inv_freq[:, None]).astype(np.float32)
    cos = np.cos(ang.astype(np.float64))
    sin = np.sin(ang.astype(np.float64))
    CS = np.zeros((128, 2, SEQ), np.float32)
    for pp in range(128):
        f = pp % 32
        CS[pp, 0] = cos[f]
        CS[pp, 1] = -sin[f] if (pp % 64) < 32 else sin[f]
    return C, CS


PO_ = {}
_c = 0
for _name, _n in (("bada", DEPTH * 48), ("norm1", DEPTH * 8), ("norm2", DEPTH * 8),
                  ("pscale", DEPTH * 4), ("fnorm", 8)):
    PO_[_name] = _c
    _c += _n
NPAR = _c


class Buf:
    __slots__ = ("name", "w", "r", "excl")

    def __init__(self, name, excl=False):
        self.name = name
        self.w = None
        self.r = {}
        self.excl = excl


class Op:
    __slots__ = ("eng", "fn", "deps", "sig", "sem", "val", "is_dma", "dsem", "batch")


class DSem:
    def __init__(self):
        self.h = None
        self.count = 0


class Prog:
    ENGS = ("pe", "act", "dve", "pool", "sp")

    def __init__(self):
        self.q = {e: [] for e in self.ENGS}
        self.dsems = []
        self.nops = 0

    def dsem(self):
        s = DSem()
        self.dsems.append(s)
        return s

    def _add(self, eng, fn, reads, writes, is_dma, dsem, batch):
        o = Op()
        o.eng = eng
        o.fn = fn
        o.sig = False
        o.sem = None
        o.val = 0
        o.is_dma = is_dma
        o.dsem = dsem
        o.batch = batch
        deps = {}

        def add(d):
            if d is None:
                return
            if (not d.is_dma) and (not is_dma) and d.eng == "pe" and eng == "pe":
                return
            deps[id(d)] = d

        for b in reads:
            add(b.w)
            if b.excl:
                for k_, x in b.r.items():
                    if k_ != eng:
                        add(x)
        for b in writes:
            add(b.w)
            for x in b.r.values():
                add(x)
        o.deps = list(deps.values())
        for d in o.deps:
            d.sig = True
        if is_dma:
            o.sig = True
        key = ("dma", id(o)) if is_dma else eng
        for b in writes:
            b.w = o
            b.r = {}
        for b in reads:
            b.r[key] = o
        self.q[eng].append(o)
        self.nops += 1
        return o

    def op(self, eng, fn, reads=(), writes=()):
        return self._add(eng, fn, reads, writes, False, None, None)

    def dma(self, eng, fn, reads, writes, dsem, batch=None):
        o = self._add(eng, fn, reads, writes, True, dsem, batch)
        if batch is not None:
            batch.append(o)
        return o

    def emit(self, nc, stack):
        esems = {}
        for e in self.ENGS:
            n = sum(1 for o in self.q[e] if o.sig and not o.is_dma)
            esems[e] = [stack.enter_context(nc.semaphore(f"s_{e}_{i}")) for i in range(n // EPOCH + 1)]
            c = 0
            for o in self.q[e]:
                if o.sig and not o.is_dma:
                    o.sem = esems[e][c // EPOCH]
                    o.val = c % EPOCH + 1
                    c += 1
        for i, s in enumerate(self.dsems):
            s.h = stack.enter_context(nc.semaphore(f"s_dma_{i}"))
        done_batches = set()
        for e in self.ENGS:
            for o in self.q[e]:
                if not o.is_dma:
                    continue
                o.sem = o.dsem.h
                if o.batch is None:
                    o.dsem.count += 16
                    o.val = o.dsem.count
                elif id(o.batch) not in done_batches:
                    done_batches.add(id(o.batch))
                    fin = o.dsem.count + 16 * len(o.batch)
                    o.dsem.count = fin
                    for x in o.batch:
                        x.val = fin
        block = stack.enter_context(nc.Block())

        def run(eng_name):
            def body(eng):
                seen = {}
                for o in self.q[eng_name]:
                    for d in o.deps:
                        k = id(d.sem)
                        if seen.get(k, 0) >= d.val:
                            continue
                        seen[k] = d.val
                        eng.wait_ge(d.sem, d.val)
                    ins = o.fn(eng)
                    if o.sig:
                        ins.then_inc(o.sem, 16 if o.is_dma else 1)
            return body

        block.tensor(run("pe"))
        block.scalar(run("act"))
        block.vector(run("dve"))
        block.gpsimd(run("pool"))
        block.sync(run("sp"))


def build_program(n_layers=DEPTH, passes=((0, 0), (0, 1), (1, 0), (1, 1))):
    nc = bass.Bass("TRN2", target_bir_lowering=False)
    P = Prog()
    stack = contextlib.ExitStack()
    n_blocks_total = n_layers * NB_LAYER

    xT = nc.dram_tensor("xT", [2, 8, 128, SEQ], F32, kind="ExternalInput").ap()
    cT = nc.dram_tensor("cT", [128, 8, 2], F32, kind="ExternalInput").ap()
    wst = nc.dram_tensor("wst", [DEPTH * NB_LAYER, 128, BLK], F32, kind="ExternalInput").ap()
    wada = nc.dram_tensor("wada", [DEPTH * 12, 128, 8 * 512], F32, kind="ExternalInput").ap()
    par = nc.dram_tensor("par", [128, NPAR], F32, kind="ExternalInput").ap()
    cst = nc.dram_tensor("cst", [128, NCONST], F32, kind="ExternalInput").ap()
    csd = nc.dram_tensor("csd", [128, 2, SEQ], F32, kind="ExternalInput").ap()
    outT = nc.dram_tensor("outT", [2, 8, 128, SEQ], F32, kind="ExternalOutput").ap()

    def sb(name, shape, dt):
        return stack.enter_context(nc.sbuf_tensor(name, shape, dt))

    x_sb = sb("x_sb", [128, 8, TT], F32)
    h_sb = sb("h_sb", [128, 8, TT], BF16)
    act_sb = sb("act_sb", [128, NFF, TT], BF16)
    rg_sb = sb("rg_sb", [128, 8, TT], BF16)
    scr_sb = sb("scr_sb", [128, NSCR, SCRW], F32)
    pl_sb = sb("pl_sb", [128, 3, 16 + TT], F32)
    cs_sb = sb("cs_sb", [128, 2, TT], F32)
    cst_sb = sb("cst_sb", [128, NCONST], F32)
    par_sb = sb("par_sb", [128, NPAR], F32)
    mod_sb = sb("mod_sb", [128, DEPTH, 48, 2], F32)
    a_sb = sb("a_sb", [128, DEPTH, 2, 8, 2], F32)
    cact_sb = sb("cact_sb", [128, 8, 2], F32)
    cbf_sb = sb("cbf_sb", [128, 2, 128], BF16)
    stg_sb = sb("stg_sb", [128, NS, BLK], F32)
    wbf_sb = sb("wbf_sb", [128, NW, BLK], BF16)
    st32_sb = sb("st32_sb", [128, DEPTH, 4, 128], F32)
    stb_sb = sb("stb_sb", [128, DEPTH, 2, 4, 128], BF16)
    carry_sb = sb("carry_sb", [128, DEPTH, 4, 16], F32)
    ps_t = [stack.enter_context(nc.psum_tensor(f"ps{i}", [128, 512], F32)) for i in range(8)]

    X = [[Buf(f"x{j}{s}") for s in range(NSUB)] for j in range(8)]
    H = [[Buf(f"h{j}{s}") for s in range(NSUB)] for j in range(8)]
    A = [[Buf(f"a{i}{s}") for s in range(NSUB)] for i in range(NFF)]
    RG = [[Buf(f"rg{j}{s}") for s in range(NSUB)] for j in range(8)]
    SCR = [Buf(f"scr{i}") for i in range(NSCR)]
    PL = [Buf(f"pl{i}") for i in range(3)]
    CSB = Buf("cs")
    CSTB = Buf("cst")
    PARB = Buf("par")
    MODB = Buf("mod")
    CACTB = Buf("cact")
    CBFB = Buf("cbf")
    STG = [Buf(f"stg{i}") for i in range(NS)]
    WBF = [Buf(f"wbf{i}") for i in range(NW)]
    ST32 = [Buf(f"st32_{l}") for l in range(DEPTH)]
    STB = [[Buf(f"stb_{l}_{i}") for i in range(2)] for l in range(DEPTH)]
    CARRY = [[Buf(f"carry_{l}_{g}") for g in range(4)] for l in range(DEPTH)]
    PS = [Buf(f"psb{i}", excl=True) for i in range(8)]

    state = {"scr": 0, "ps": 0}

    def scr():
        i = state["scr"]
        state["scr"] = (i + 1) % NSCR
        return scr_sb[:, i, :], SCR[i]

    def psum():
        i = state["ps"]
        state["ps"] = (i + 1) % 8
        return ps_t[i][:], PS[i]

    def cview(name, n):
        return cst_sb[:, CO[name]:CO[name] + n]

    class WS:
        def __init__(self):
            self.next_dma = 0
            self.next_cast = 0
            self.maxb = -1
            self.sems = [P.dsem() for _ in range(NS)]
            self.order = []
            self.ncast = 0

        def set_order(self, order):
            self.order = order

        def _dma(self, b):
            if b >= len(self.order):
                return
            slot = b % NS
            src = wst[self.order[b]]
            P.dma("sp", lambda e, slot=slot, src=src: e.dma_start(out=stg_sb[:, slot, :], in_=src),
                  reads=[], writes=[STG[slot]], dsem=self.sems[slot])

        def _cast(self, b):
            while self.next_dma < b + NS:
                self._dma(self.next_dma)
                self.next_dma += 1
            ss, ws = b % NS, b % NW
            eng = ("act", "dve", "act", "dve", "pool")[self.ncast % 5]
            self.ncast += 1
            if eng == "dve":
                P.op("dve", lambda e, ss=ss, ws=ws: e.tensor_copy(out=wbf_sb[:, ws, :], in_=stg_sb[:, ss, :]),
                     reads=[STG[ss]], writes=[WBF[ws]])
            elif eng == "pool":
                P.op("pool", lambda e, ss=ss, ws=ws: e.tensor_copy(out=wbf_sb[:, ws, :], in_=stg_sb[:, ss, :]),
                     reads=[STG[ss]], writes=[WBF[ws]])
            else:
                P.op("act", lambda e, ss=ss, ws=ws: e.copy(out=wbf_sb[:, ws, :], in_=stg_sb[:, ss, :]),
                     reads=[STG[ss]], writes=[WBF[ws]])
            if self.next_dma == b + NS:
                self._dma(self.next_dma)
                self.next_dma += 1

        def touch(self, b):
            tgt = min(b + LOOKAHEAD, len(self.order) - 1)
            while self.next_cast <= tgt:
                self._cast(self.next_cast)
                self.next_cast += 1
            if b > self.maxb:
                self.maxb = b
            assert b > self.maxb - WINDOW, (b, self.maxb)

        def tile(self, base_blk, t, n=1):
            b = base_blk + t // BLK_TILES
            assert (t % BLK_TILES) + n <= BLK_TILES
            self.touch(b)
            ws = b % NW
            c0 = (t % BLK_TILES) * 128
            return wbf_sb[:, ws, c0:c0 + 128 * n], WBF[ws]

    ws = WS()
    order = []
    for (sq, hf) in passes:
        for l in range(n_layers):
            order.extend(range(l * NB_LAYER, (l + 1) * NB_LAYER))
    ws.set_order(order)

    ld_sem = P.dsem()
    ldb = []
    P.dma("sp", lambda e: e.dma_start(out=cst_sb[:], in_=cst[:, :]), [], [CSTB], ld_sem, ldb)
    P.dma("sp", lambda e: e.dma_start(out=par_sb[:], in_=par[:, :]), [], [PARB], ld_sem, ldb)
    P.dma("sp", lambda e: e.dma_start(out=cact_sb[:], in_=cT[:, :, :]), [], [CACTB], ld_sem, ldb)
    P.op("dve", lambda e: e.tensor_copy(out=cbf_sb[:, 0, :], in_=cview("ident", 128)), [CSTB], [CBFB])
    P.op("dve", lambda e: e.tensor_copy(out=cbf_sb[:, 1, :], in_=cview("pswap", 128)), [CBFB, CSTB], [CBFB])
    P.op("act", lambda e: e.activation(out=cact_sb[:], in_=cact_sb[:], func=AF.Silu), [CACTB], [CACTB])
    ident = cbf_sb[:, 0, :]
    pswap = cbf_sb[:, 1, :]
    onesd = cview("onesd", 128)
    onesh = cview("onesh", 128)

    wa_f32 = act_sb[:].rearrange("p a b -> p (a b)").bitcast(F32)
    WA = [Buf("wa0"), Buf("wa1")]
    wa_sems = [P.dsem(), P.dsem()]
    allA = [A[i][s] for i in range(NFF) for s in range(NSUB)]
    for l in range(n_layers):
        mps, mpsb = psum()
        for pc in range(12):
            slot = (l * 12 + pc) % 2
            src = wada[l * 12 + pc]
            P.dma("sp", lambda e, slot=slot, src=src: e.dma_start(out=wa_f32[:, slot * 4096:(slot + 1) * 4096], in_=src),
                  [], [WA[slot]], wa_sems[slot])
            for ct in range(4):
                t = pc * 4 + ct
                for kk in range(8):
                    P.op("pe", lambda e, slot=slot, ct=ct, kk=kk, t=t, mps=mps: e.matmul(
                        mps[:, 2 * t:2 * t + 2],
                        wa_f32[:, slot * 4096 + kk * 512 + ct * 128: slot * 4096 + kk * 512 + ct * 128 + 128],
                        cact_sb[:, kk, :], start=(kk == 0), stop=(kk == 7)),
                        [WA[slot], CACTB], [mpsb])
        bada = par_sb[:, PO_["bada"] + l * 48: PO_["bada"] + (l + 1) * 48]
        P.op("dve", lambda e, l=l, mps=mps, bada=bada: e.tensor_tensor(
            out=mod_sb[:, l, :, :], in0=mps[:, 0:96].rearrange("p (t b) -> p t b", b=2),
            in1=bada.unsqueeze(2).broadcast_to([128, 48, 2]), op=ALU.add), [mpsb, PARB], [MODB])
        for k, (t0, nm) in enumerate(((8, "norm1"), (32, "norm2"))):
            nv = par_sb[:, PO_[nm] + l * 8: PO_[nm] + (l + 1) * 8]
            P.op("dve", lambda e, l=l, k=k, t0=t0, nv=nv: e.scalar_tensor_tensor(
                out=a_sb[:, l, k, :, :], in0=mod_sb[:, l, t0:t0 + 8, :], scalar=1.0,
                in1=nv.unsqueeze(2).broadcast_to([128, 8, 2]), op0=ALU.add, op1=ALU.mult), [MODB, PARB], [MODB])
    P.op("pool", lambda e: e.memset(act_sb[:, 0, 0:16], 0.0), [], allA + WA)

    def modv(l, t, b):
        return mod_sb[:, l, t, b:b + 1]

    def rmsnorm_to_h(l, bsel, which):
        tB = 0 if which == 0 else 24
        for s in range(NSUB):
            sl = slice(s * 512, (s + 1) * 512)
            stp, stb_ = psum()
            for j in range(8):
                sq, sqb = scr()
                P.op("act", lambda e, j=j, sq=sq, sl=sl: e.activation(out=sq[:, 0:512], in_=x_sb[:, j, sl], func=AF.Square),
                     [X[j][s]], [sqb])
                P.op("pe", lambda e, j=j, sq=sq, stp=stp: e.matmul(stp, onesd, sq[:, 0:512], start=(j == 0), stop=(j == 7)),
                     [sqb, CSTB], [stb_])
            rs, rsb = scr()
            P.op("act", lambda e, rs=rs, stp=stp: e.activation(out=rs[:, 0:512], in_=stp, func=AF.Sqrt, bias=EPS, scale=1.0),
                 [stb_], [rsb])
            P.op("dve", lambda e, rs=rs: e.reciprocal(out=rs[:, 0:512], in_=rs[:, 0:512]), [rsb], [rsb])
            for j in range(8):
                tm, tmb = scr()
                P.op("dve", lambda e, j=j, tm=tm, rs=rs, sl=sl: e.tensor_tensor(
                    out=tm[:, 0:512], in0=x_sb[:, j, sl], in1=rs[:, 0:512], op=ALU.mult), [X[j][s], rsb], [tmb])
                P.op("act", lambda e, j=j, tm=tm, sl=sl: e.activation(
                    out=h_sb[:, j, sl], in_=tm[:, 0:512], func=AF.Identity,
                    bias=modv(l, tB + j, bsel), scale=a_sb[:, l, which, j, bsel:bsel + 1]),
                    [tmb, MODB], [H[j][s]])

    def proj_fm(base, t0, s, nk=8, rhs_of=None):
        ps, psb = psum()
        sl = slice(s * 512, (s + 1) * 512)
        for kk in range(nk):
            w, wb = ws.tile(base, t0 + kk)
            r, rb = rhs_of(kk, s, sl)
            P.op("pe", lambda e, ps=ps, w=w, r=r, kk=kk: e.matmul(ps, w, r, start=(kk == 0), stop=(kk == nk - 1)),
                 [wb, rb], [psb])
        return ps, psb

    def rhs_h(kk, s, sl):
        return h_sb[:, kk, sl], H[kk][s]

    def rhs_rg(kk, s, sl):
        return rg_sb[:, kk, sl], RG[kk][s]

    def rhs_a(off):
        def f(kk, s, sl):
            return act_sb[:, off + kk, sl], A[off + kk][s]
        return f

    def layer(l, bsel, hf, base):
        pos0 = hf * TT
        rmsnorm_to_h(l, bsel, 0)

        if DEBUG_STOP == 1:
            return
        def rot_stage2(part, j, s, sl, rawbf, rawb, t1, t1b):
            ps2, ps2b = psum()
            P.op("pe", lambda e: e.matmul(ps2, pswap, rawbf, start=True, stop=True), [rawb, CBFB], [ps2b])
            t2, t2b = scr()
            P.op("dve", lambda e: e.tensor_tensor(out=t2[:, 0:512], in0=ps2, in1=cs_sb[:, 1, sl], op=ALU.mult),
                 [ps2b, CSB], [t2b])
            if part == 1:
                P.op("pool", lambda e: e.tensor_tensor(
                    out=act_sb[:, 4 + j, sl], in0=t1[:, 0:512], in1=t2[:, 0:512], op=ALU.add),
                    [t1b, t2b], [A[4 + j][s]])
            else:
                P.op("pool", lambda e: e.tensor_tensor(
                    out=t1[:, 0:512], in0=t1[:, 0:512], in1=t2[:, 0:512], op=ALU.add), [t1b, t2b], [t1b])
                qd = cst_sb[:, CO["qdec"] + j * 128: CO["qdec"] + (j + 1) * 128]
                P.op("pool", lambda e: e.tensor_tensor(
                    out=act_sb[:, j, sl].rearrange("p (a b) -> p a b", a=4),
                    in0=t1[:, 0:512].rearrange("p (a b) -> p a b", a=4),
                    in1=qd.unsqueeze(1).broadcast_to([128, 4, 128]), op=ALU.mult),
                    [t1b, CSTB], [A[j][s]])

        pending = None
        for part in range(2):
            for j in range(4):
                for s in range(NSUB):
                    sl = slice(s * 512, (s + 1) * 512)
                    ps, psb = proj_fm(base, OFF["q" if part == 0 else "k"] + j * 8, s, 8, rhs_h)
                    raw, rawb = scr()
                    rawbf = raw.bitcast(BF16)[:, 0:512]
                    P.op("act", lambda e, rawbf=rawbf, ps=ps: e.copy(out=rawbf, in_=ps), [psb], [rawb])
                    t1, t1b = scr()
                    P.op("dve", lambda e, t1=t1, ps=ps, sl=sl: e.tensor_tensor(
                        out=t1[:, 0:512], in0=ps, in1=cs_sb[:, 0, sl], op=ALU.mult), [psb, CSB], [t1b])
                    if pending is not None:
                        rot_stage2(*pending)
                    pending = (part, j, s, sl, rawbf, rawb, t1, t1b)
        rot_stage2(*pending)

        kdec = cview("kdec", 8)

        def k_transpose(n):
            s, c0 = n // 4, (n % 4) * 128
            ps, psb = psum()
            psbf = ps.bitcast(BF16)
            for j in range(4):
                P.op("pe", lambda e, j=j: e.transpose(
                    psbf[:, j * 128:(j + 1) * 128], act_sb[:, 4 + j, s * 512 + c0: s * 512 + c0 + 128], ident),
                    [A[4 + j][s], CBFB], [psb])
            P.op("dve", lambda e: e.tensor_tensor(
                out=act_sb[:, 16 + n // 2, (n % 2) * 512:(n % 2 + 1) * 512].rearrange("p (h a) -> p h a", h=8),
                in0=psbf[:, 0:512].rearrange("p (h a) -> p h a", h=8),
                in1=kdec.unsqueeze(2).broadcast_to([128, 8, 64]), op=ALU.mult),
                [psb, CSTB], [A[16 + n // 2][n % 2]])

        gi = 0
        for cg in range(2):
            for n in range(NCH):
                s, c0 = n // 4, (n % 4) * 128
                ps, psb = psum()
                for kk in range(8):
                    w, wb = ws.tile(base, OFF["v"] + (cg * 8 + kk) * 4, 4)
                    P.op("pe", lambda e, ps=ps, w=w, kk=kk, s=s, c0=c0: e.matmul(
                        ps, h_sb[:, kk, s * 512 + c0: s * 512 + c0 + 128], w, start=(kk == 0), stop=(kk == 7)),
                        [wb, H[kk][s]], [psb])
                P.op("dve", lambda e, ps=ps, n=n, cg=cg: e.tensor_copy(out=act_sb[:, 8 + n, cg * 512:(cg + 1) * 512], in_=ps),
                     [psb], [A[8 + n][cg]])
                if gi % 2 == 1:
                    k_transpose(gi // 2)
                gi += 1

        if hf == 0:
            P.op("pool", lambda e: e.memset(st32_sb[:, l, :, :], 0.0), [], [ST32[l]])
            P.op("pool", lambda e: e.memset(stb_sb[:, l, 0, :, :], 0.0), [], [STB[l][0]])

        mask = cview("mask", 1024)
        cdec = cview("cdec", 4)
        mask4 = mask.rearrange("p (j q c) -> p j q c", j=4, q=2)
        rg4 = rg_sb[:].rearrange("p (j q) t -> p j q t", q=2)
        for n in range(NCH):
            s, c0 = n // 4, (n % 4) * 128
            tsl = slice(s * 512 + c0, s * 512 + c0 + 128)
            vbufs = [A[8 + n][0], A[8 + n][1]]
            cur, nxt = n % 2, (n + 1) % 2
            sc_ps = [psum(), psum()]
            for h in range(8):
                j, par = h // 2, h % 2
                p0 = par * 64
                bank, bankb = sc_ps[par]
                P.op("pe", lambda e, bank=bank, j=j, p0=p0, tsl=tsl: e.matmul(
                    bank[:, j * 128:(j + 1) * 128], act_sb[p0:p0 + 64, 4 + j, tsl], act_sb[p0:p0 + 64, j, tsl],
                    start=True, stop=True), [A[4 + j][s], A[j][s]], [bankb])
            pkv, pkvb = psum()
            kdb = A[16 + n // 2][n % 2]
            for h in range(8):
                P.op("pe", lambda e, pkv=pkv, h=h, n=n: e.matmul(
                    pkv[(h % 2) * 64:(h % 2) * 64 + 64, (h // 2) * 128:(h // 2 + 1) * 128],
                    act_sb[:, 16 + n // 2, (n % 2) * 512 + h * 64:(n % 2) * 512 + (h + 1) * 64],
                    act_sb[:, 8 + n, h * 128:(h + 1) * 128], start=True, stop=True),
                    [kdb, vbufs[h // 4]], [pkvb])
            stts = []
            for par in range(2):
                bank, bankb = sc_ps[par]
                stt, sttb = scr()
                sttbf = stt.bitcast(BF16)[:, 0:512]
                P.op("dve", lambda e, sttbf=sttbf, bank=bank, par=par: e.tensor_tensor(
                    out=sttbf.rearrange("p (j c) -> p j c", j=4), in0=bank.rearrange("p (j c) -> p j c", j=4),
                    in1=mask4[:, :, par, :], op=ALU.mult), [bankb, CSTB], [sttb])
                stts.append((sttbf, sttb))
            tmp, tmpb = scr()
            P.op("pool", lambda e, tmp=tmp: e.tensor_tensor(
                out=tmp[:, 0:512].rearrange("p (a b) -> p a b", a=4), in0=st32_sb[:, l, :, :],
                in1=cdec.unsqueeze(2).broadcast_to([128, 4, 128]), op=ALU.mult), [ST32[l], CSTB], [tmpb])
            P.op("dve", lambda e, tmp=tmp, pkv=pkv: e.tensor_tensor(
                out=st32_sb[:, l, :, :], in0=tmp[:, 0:512].rearrange("p (a b) -> p a b", a=4),
                in1=pkv.rearrange("p (a b) -> p a b", a=4), op=ALU.add), [tmpb, pkvb], [ST32[l]])
            P.op("act", lambda e, nxt=nxt: e.copy(out=stb_sb[:, l, nxt, :, :], in_=st32_sb[:, l, :, :]),
                 [ST32[l]], [STB[l][nxt]])
            o_ps = [psum(), psum()]
            for h in range(8):
                j, par = h // 2, h % 2
                p0 = par * 64
                bank, bankb = o_ps[par]
                sttbf, sttb = stts[par]
                P.op("pe", lambda e, bank=bank, j=j, h=h, n=n, sttbf=sttbf: e.matmul(
                    bank[:, j * 128:(j + 1) * 128], act_sb[:, 8 + n, h * 128:(h + 1) * 128],
                    sttbf[:, j * 128:(j + 1) * 128], start=True, stop=False),
                    [vbufs[h // 4], sttb], [bankb])
                P.op("pe", lambda e, bank=bank, j=j, p0=p0, tsl=tsl, cur=cur: e.matmul(
                    bank[:, j * 128:(j + 1) * 128], stb_sb[p0:p0 + 64, l, cur, j, :],
                    act_sb[p0:p0 + 64, j, tsl], start=False, stop=True),
                    [STB[l][cur], A[j][s]], [bankb])
            for par in range(2):
                bank, bankb = o_ps[par]
                osq, osqb = scr()
                P.op("act", lambda e, osq=osq, bank=bank: e.activation(out=osq[:, 0:512], in_=bank, func=AF.Square),
                     [bankb], [osqb])
                psn, psnb = psum()
                P.op("pe", lambda e, psn=psn, osq=osq: e.matmul(psn, onesh, osq[:, 0:512], start=True, stop=True),
                     [osqb, CSTB], [psnb])
                hr, hrb = scr()
                P.op("act", lambda e, hr=hr, psn=psn: e.activation(out=hr[:, 0:512], in_=psn, func=AF.Sqrt, bias=EPS, scale=1.0),
                     [psnb], [hrb])
                P.op("dve", lambda e, hr=hr: e.reciprocal(out=hr[:, 0:512], in_=hr[:, 0:512]), [hrb], [hrb])
                P.op("dve", lambda e, hr=hr, bank=bank, par=par, tsl=tsl: e.tensor_tensor(
                    out=rg4[:, :, par, tsl], in0=bank.rearrange("p (a b) -> p a b", a=4),
                    in1=hr[:, 0:512].rearrange("p (a b) -> p a b", a=4), op=ALU.mult),
                    [hrb, bankb], [RG[2 * i + par][s] for i in range(4)])

        if DEBUG_STOP == 5:
            return
        invc = cview("invcnt", 64)
        for g in range(4):
            w = 2 << g
            for s in range(NSUB):
                ps, psb = proj_fm(base, OFF["p"] + g * 8, s, 8, rhs_h)
                P.op("act", lambda e, ps=ps, s=s: e.copy(out=pl_sb[:, 0, 16 + s * 512:16 + (s + 1) * 512], in_=ps),
                     [psb], [PL[0]])
            if hf == 0:
                P.op("pool", lambda e: e.memset(pl_sb[:, 0, 0:16], 0.0), [], [PL[0]])
            else:
                P.op("pool", lambda e, g=g: e.tensor_copy(out=pl_sb[:, 0, 0:16], in_=carry_sb[:, l, g, :]),
                     [CARRY[l][g]], [PL[0]])
            P.op("pool", lambda e, g=g: e.tensor_copy(out=carry_sb[:, l, g, :], in_=pl_sb[:, 0, TT:TT + 16]),
                 [PL[0]], [CARRY[l][g]])
            cur, curb = 0, PL[0]
            sh, lo = 1, 0
            while sh < w:
                nxt = 1 if cur != 1 else 2
                lo2 = lo + sh
                P.op("pool", lambda e, cur=cur, nxt=nxt, sh=sh, lo2=lo2: e.tensor_tensor(
                    out=pl_sb[:, nxt, lo2:16 + TT], in0=pl_sb[:, cur, lo2:16 + TT],
                    in1=pl_sb[:, cur, lo2 - sh:16 + TT - sh], op=ALU.add), [PL[cur]], [PL[nxt]])
                cur, lo, sh = nxt, lo2, sh * 2
            P.op("dve", lambda e, cur=cur, g=g, w=w: e.scalar_tensor_tensor(
                out=act_sb[:, 8 + g, :], in0=pl_sb[:, cur, 16:16 + TT], scalar=1.0 / w,
                in1=pl_sb[:, 0, 16:16 + TT], op0=ALU.mult, op1=ALU.subtract), [PL[cur], PL[0]], [A[8 + g][0], A[8 + g][1]])
            if hf == 0:
                tm, tmb = scr()
                P.op("dve", lambda e, tm=tm, cur=cur, g=g: e.tensor_tensor(
                    out=tm[:, 0:16], in0=pl_sb[:, cur, 16:32], in1=invc[:, g * 16:(g + 1) * 16], op=ALU.mult),
                    [PL[cur], CSTB], [tmb])
                P.op("dve", lambda e, tm=tm, g=g: e.tensor_tensor(
                    out=act_sb[:, 8 + g, 0:16], in0=tm[:, 0:16], in1=pl_sb[:, 0, 16:32], op=ALU.subtract),
                    [tmb, PL[0], A[8 + g][0]], [A[8 + g][0]])
        for h in range(8):
            for s in range(NSUB):
                sl = slice(s * 512, (s + 1) * 512)
                ps, psb = proj_fm(base, OFF["g"] + h * 8, s, 8, rhs_h)
                sg, sgb = scr()
                sgbf = sg.bitcast(BF16)[:, 0:512]
                P.op("act", lambda e, sgbf=sgbf, ps=ps: e.activation(out=sgbf, in_=ps, func=AF.Silu), [psb], [sgb])
                P.op("pool", lambda e, h=h, sl=sl, sgbf=sgbf: e.tensor_tensor(
                    out=rg_sb[:, h, sl], in0=rg_sb[:, h, sl], in1=sgbf, op=ALU.mult), [sgb, RG[h][s]], [RG[h][s]])

        if DEBUG_STOP == 6:
            return
        for g in range(4):
            for s in range(NSUB):
                sl = slice(s * 512, (s + 1) * 512)
                ps, psb = psum()
                wt, wtb = ws.tile(base, OFF["grp"] + g)
                P.op("pe", lambda e, ps=ps, wt=wt, g=g, sl=sl: e.matmul(ps, wt, act_sb[:, 8 + g, sl], start=True, stop=True),
                     [wtb, A[8 + g][s]], [psb])
                psc = par_sb[:, PO_["pscale"] + l * 4 + g: PO_["pscale"] + l * 4 + g + 1]
                P.op("act", lambda e, ps=ps, g=g, sl=sl, psc=psc: e.activation(
                    out=act_sb[:, 12 + g, sl], in_=ps, func=AF.Identity, bias=0.0, scale=psc), [psb, PARB], [A[12 + g][s]])

        if DEBUG_STOP == 7:
            return
        for j in range(8):
            m0 = OFF["mrg"] + j * 28
            for s in range(NSUB):
                sl = slice(s * 512, (s + 1) * 512)
                prd, prdb = proj_fm(base, m0, s, 8, rhs_rg)
                ppd, ppdb = proj_fm(base, m0 + 8, s, 4, rhs_a(12))
                par_, parb_ = proj_fm(base, m0 + 12, s, 8, rhs_h)
                pap, papb = proj_fm(base, m0 + 20, s, 8, rhs_h)
                sr, srb = scr()
                P.op("act", lambda e, sr=sr, par_=par_: e.activation(out=sr[:, 0:512], in_=par_, func=AF.Sigmoid), [parb_], [srb])
                sp_, spb = scr()
                P.op("act", lambda e, sp_=sp_, pap=pap: e.activation(out=sp_[:, 0:512], in_=pap, func=AF.Sigmoid), [papb], [spb])
                P.op("dve", lambda e, sr=sr, prd=prd: e.tensor_tensor(out=sr[:, 0:512], in0=prd, in1=sr[:, 0:512], op=ALU.mult),
                     [prdb, srb], [srb])
                P.op("dve", lambda e, sp_=sp_, ppd=ppd: e.tensor_tensor(out=sp_[:, 0:512], in0=ppd, in1=sp_[:, 0:512], op=ALU.mult),
                     [ppdb, spb], [spb])
                P.op("pool", lambda e, j=j, sl=sl, sr=sr, sp_=sp_: e.tensor_tensor(
                    out=act_sb[:, j, sl], in0=sr[:, 0:512], in1=sp_[:, 0:512], op=ALU.add), [srb, spb], [A[j][s]])

        if DEBUG_STOP == 8:
            return
        for j in range(8):
            for s in range(NSUB):
                sl = slice(s * 512, (s + 1) * 512)
                ps, psb = proj_fm(base, OFF["out"] + j * 8, s, 8, rhs_a(0))
                P.op("dve", lambda e, ps=ps, j=j, sl=sl: e.scalar_tensor_tensor(
                    out=x_sb[:, j, sl], in0=ps, scalar=modv(l, 16 + j, bsel), in1=x_sb[:, j, sl],
                    op0=ALU.mult, op1=ALU.add), [psb, MODB, X[j][s]], [X[j][s]])

        if DEBUG_STOP == 9:
            return
        rmsnorm_to_h(l, bsel, 1)
        for i in range(NFF):
            for s in range(NSUB):
                sl = slice(s * 512, (s + 1) * 512)
                pg, pgb = proj_fm(base, OFF["ffi"] + i * 16, s, 8, rhs_h)
                pu, pub = proj_fm(base, OFF["ffi"] + i * 16 + 8, s, 8, rhs_h)
                sg, sgb = scr()
                P.op("act", lambda e, sg=sg, pg=pg: e.activation(out=sg[:, 0:512], in_=pg, func=AF.Silu), [pgb], [sgb])
                P.op("dve", lambda e, sg=sg, pu=pu, i=i, sl=sl: e.tensor_tensor(
                    out=act_sb[:, i, sl], in0=pu, in1=sg[:, 0:512], op=ALU.mult), [pub, sgb], [A[i][s]])
        for j in range(8):
            for s in range(NSUB):
                sl = slice(s * 512, (s + 1) * 512)
                ps, psb = proj_fm(base, OFF["ffo"] + j * NFF, s, NFF, rhs_a(0))
                P.op("dve", lambda e, ps=ps, j=j, sl=sl: e.scalar_tensor_tensor(
                    out=x_sb[:, j, sl], in0=ps, scalar=modv(l, 40 + j, bsel), in1=x_sb[:, j, sl],
                    op0=ALU.mult, op1=ALU.add), [psb, MODB, X[j][s]], [X[j][s]])

    xl_sem = P.dsem()
    st_sem = P.dsem()
    cs_sem = P.dsem()
    store_ops = []
    blk = 0
    for (sq, hf) in passes:
        pos0 = hf * TT
        lb = []
        for j in range(8):
            for s in range(NSUB):
                P.dma("act", lambda e, j=j, s=s, sq=sq, pos0=pos0: e.dma_start(
                    out=x_sb[:, j, s * 512:(s + 1) * 512], in_=xT[sq, j, :, pos0 + s * 512: pos0 + (s + 1) * 512]),
                    [], [X[j][s]], xl_sem, lb)
        P.dma("act", lambda e, pos0=pos0: e.dma_start(out=cs_sb[:], in_=csd[:, :, pos0:pos0 + TT]), [], [CSB], cs_sem)
        for l in range(n_layers):
            layer(l, sq, hf, blk)
            blk += NB_LAYER
        fn = par_sb[:, PO_["fnorm"]: PO_["fnorm"] + 8]
        sbatch = []
        for s in range(NSUB):
            sl = slice(s * 512, (s + 1) * 512)
            stp, stb_ = psum()
            for j in range(8):
                sq_, sqb = scr()
                P.op("act", lambda e, j=j, sq_=sq_, sl=sl: e.activation(out=sq_[:, 0:512], in_=x_sb[:, j, sl], func=AF.Square),
                     [X[j][s]], [sqb])
                P.op("pe", lambda e, j=j, sq_=sq_, stp=stp: e.matmul(stp, onesd, sq_[:, 0:512], start=(j == 0), stop=(j == 7)),
                     [sqb, CSTB], [stb_])
            rs, rsb = scr()
            P.op("act", lambda e, rs=rs, stp=stp: e.activation(out=rs[:, 0:512], in_=stp, func=AF.Sqrt, bias=EPS, scale=1.0),
                 [stb_], [rsb])
            P.op("dve", lambda e, rs=rs: e.reciprocal(out=rs[:, 0:512], in_=rs[:, 0:512]), [rsb], [rsb])
            for j in range(8):
                P.op("dve", lambda e, j=j, rs=rs, sl=sl: e.scalar_tensor_tensor(
                    out=x_sb[:, j, sl], in0=x_sb[:, j, sl], scalar=fn[:, j:j + 1], in1=rs[:, 0:512],
                    op0=ALU.mult, op1=ALU.mult), [X[j][s], rsb, PARB], [X[j][s]])
                o = P.dma("act", lambda e, j=j, s=s, sq=sq, pos0=pos0, sl=sl: e.dma_start(
                    out=outT[sq, j, :, pos0 + s * 512: pos0 + (s + 1) * 512], in_=x_sb[:, j, sl]),
                    [X[j][s]], [], st_sem, sbatch)
                store_ops.append(o)
    fin = P.op("act", lambda e: e.activation(out=scr_sb[:, 0, 0:8], in_=scr_sb[:, 0, 0:8], func=AF.Copy), [], [SCR[0]])
    dd = {id(o): o for o in fin.deps}
    for o in store_ops:
        dd[id(o)] = o
    fin.deps = list(dd.values())

    P.emit(nc, stack)
    return nc, stack, P


def prep_shared(w_ada, b_ada, norm1, w_in, w_ret_o, w_pool_grp, pool_scale, w_pool_o,
                w_out, norm2, w_ffn_in, w_ffn_out, final_norm):
    f = lambda a: np.ascontiguousarray(np.asarray(a, dtype=np.float32))
    wst = np.concatenate([pack_layer(f(w_in[l]), f(w_ret_o[l]), f(w_pool_grp[l]), f(w_pool_o[l]), f(w_out[l]),
                                     f(w_ffn_in[l]), f(w_ffn_out[l])) for l in range(DEPTH)], axis=0)
    wa = f(w_ada).reshape(DEPTH, 8, 128, 12, 512).transpose(0, 3, 2, 1, 4).reshape(DEPTH * 12, 128, 8 * 512)
    par = np.zeros((128, NPAR), np.float32)
    par[:, PO_["bada"]:PO_["bada"] + DEPTH * 48] = f(b_ada).reshape(DEPTH, 48, 128).transpose(2, 0, 1).reshape(128, -1)
    par[:, PO_["norm1"]:PO_["norm1"] + DEPTH * 8] = f(norm1).reshape(DEPTH, 8, 128).transpose(2, 0, 1).reshape(128, -1)
    par[:, PO_["norm2"]:PO_["norm2"] + DEPTH * 8] = f(norm2).reshape(DEPTH, 8, 128).transpose(2, 0, 1).reshape(128, -1)
    par[:, PO_["pscale"]:PO_["pscale"] + DEPTH * 4] = f(pool_scale).reshape(DEPTH, 4, 128).transpose(2, 0, 1).reshape(128, -1)
    par[:, PO_["fnorm"]:PO_["fnorm"] + 8] = f(final_norm).reshape(8, 128).T
    cst, csd = make_consts()
    return {"wst": np.ascontiguousarray(wst), "wada": np.ascontiguousarray(wa), "par": par, "cst": cst, "csd": csd}


def prep_core(x, c, core):
    xs = np.asarray(x[2 * core:2 * core + 2], dtype=np.float32)
    xT = np.ascontiguousarray(xs.transpose(0, 2, 1).reshape(2, 8, 128, SEQ))
    cs = np.asarray(c[2 * core:2 * core + 2], dtype=np.float32)
    cT = np.ascontiguousarray(cs.reshape(2, 8, 128).transpose(2, 1, 0))
    return {"xT": xT, "cT": cT}


_CACHE = {}


def kernel(x, c, w_ada, b_ada, norm1, w_in, w_ret_o, w_pool_grp, pool_scale, w_pool_o,
           w_out, norm2, w_ffn_in, w_ffn_out, final_norm):
    shared = prep_shared(w_ada, b_ada, norm1, w_in, w_ret_o, w_pool_grp, pool_scale, w_pool_o,
                         w_out, norm2, w_ffn_in, w_ffn_out, final_norm)
    nc, stack, P = build_program()
    in_maps = []
    for core in range(NCORES):
        m = dict(shared)
        m.update(prep_core(x, c, core))
        in_maps.append(m)
    res = run_bass_kernel_spmd(nc, in_maps, core_ids=list(range(NCORES)))
    out = np.empty((BATCH, SEQ, D), np.float32)
    for core in range(NCORES):
        o = np.asarray(res.results[core]["outT"]).reshape(2, D, SEQ)
        out[2 * core:2 * core + 2] = o.transpose(0, 2, 1)
    return out
```

```python
import contextlib
import numpy as np
import concourse.bass as bass
import concourse.mybir as mybir
from concourse.bass_utils import run_bass_kernel_spmd

F32 = mybir.dt.float32
BF16 = mybir.dt.bfloat16
AF = mybir.ActivationFunctionType
ALU = mybir.AluOpType

D = 1024
SEQ = 2048
BATCH = 16
DEPTH = 4
NCORES = 8
TT = 1024
NSUB = 2
NCH = 8
DFF = 2816
NFF = 22
EPS = 1e-6
BLK_TILES = 8
BLK = BLK_TILES * 128
NS = 3
NW = 8
LOOKAHEAD = 3
WINDOW = NW - LOOKAHEAD
NSCR = 11
SCRW = 528
EPOCH = 12000
DEBUG_STOP = 0
DEBUG_VAR = 0

OFF = {}
_o = 0
for _name, _n in (("q", 32), ("k", 32), ("v", 64), ("g", 64), ("p", 32), ("grp", 4),
                  ("mrg", 8 * 28), ("out", 64), ("ffi", NFF * 16), ("ffo", 8 * NFF)):
    OFF[_name] = _o
    _o += _n
NT_LAYER = _o
NB_LAYER = -(-NT_LAYER // BLK_TILES)
assert OFF["v"] % 4 == 0


def _tiles_of(W):
    K, N = W.shape
    return W.reshape(K // 128, 128, N // 128, 128).transpose(2, 0, 1, 3)


def pack_layer(w_in, w_ret_o, w_pool_grp, w_pool_o, w_out, w_ffn_in, w_ffn_out):
    T = np.zeros((NB_LAYER * BLK_TILES, 128, 128), np.float32)
    tin = _tiles_of(w_in)
    T[OFF["q"]:OFF["q"] + 32] = tin[0:4].reshape(32, 128, 128)
    T[OFF["k"]:OFF["k"] + 32] = tin[4:8].reshape(32, 128, 128)
    tv = tin[8:16].reshape(2, 4, 8, 128, 128).transpose(0, 2, 1, 3, 4)
    T[OFF["v"]:OFF["v"] + 64] = tv.reshape(64, 128, 128)
    T[OFF["g"]:OFF["g"] + 64] = tin[16:24].reshape(64, 128, 128)
    T[OFF["p"]:OFF["p"] + 32] = tin[24:28].reshape(32, 128, 128)
    T[OFF["grp"]:OFF["grp"] + 4] = w_pool_grp
    tro = _tiles_of(w_ret_o)
    tpo = _tiles_of(w_pool_o)
    m = OFF["mrg"]
    for j in range(8):
        T[m:m + 8] = tro[j]
        T[m + 8:m + 12] = tpo[j]
        T[m + 12:m + 20] = tin[28 + j]
        T[m + 20:m + 28] = tin[36 + j]
        m += 28
    T[OFF["out"]:OFF["out"] + 64] = _tiles_of(w_out).reshape(64, 128, 128)
    tfi = _tiles_of(w_ffn_in)
    f = OFF["ffi"]
    for i in range(NFF):
        T[f:f + 8] = tfi[i]
        T[f + 8:f + 16] = tfi[NFF + i]
        f += 16
    T[OFF["ffo"]:OFF["ffo"] + 8 * NFF] = _tiles_of(w_ffn_out).reshape(8 * NFF, 128, 128)
    return T.reshape(NB_LAYER, BLK_TILES, 128, 128).transpose(0, 2, 1, 3).reshape(NB_LAYER, 128, BLK)


CO = {}
_c = 0
for _name, _n in (("mask", 1024), ("qdec", 512), ("kdec", 8), ("cdec", 4), ("invcnt", 64),
                  ("ident", 128), ("pswap", 128), ("onesd", 128), ("onesh", 128)):
    CO[_name] = _c
    _c += _n
NCONST = _c


def make_consts():
    C = np.zeros((128, NCONST), np.float64)
    gam = 1.0 - 2.0 ** (-5.0 - np.arange(8))
    p = np.arange(128)
    idx = np.arange(128)
    msk = np.zeros((128, 8, 128))
    for h in range(8):
        msk[:, h, :] = (idx[None, :] >= idx[:, None]) * (gam[h] ** (-(idx[:, None] + 1.0)))
    C[:, CO["mask"]:CO["mask"] + 1024] = msk.reshape(128, 1024)
    qd = np.zeros((128, 4, 128))
    for j in range(4):
        for half in range(2):
            qd[half * 64:(half + 1) * 64, j, :] = (64 ** -0.5) * gam[2 * j + half] ** (idx[None, :] + 1.0)
    C[:, CO["qdec"]:CO["qdec"] + 512] = qd.reshape(128, 512)
    for h in range(8):
        C[:, CO["kdec"] + h] = gam[h] ** (127.0 - p)
    for j in range(4):
        C[0:64, CO["cdec"] + j] = gam[2 * j] ** 128.0
        C[64:128, CO["cdec"] + j] = gam[2 * j + 1] ** 128.0
    for g, w in enumerate((2, 4, 8, 16)):
        C[:, CO["invcnt"] + g * 16:CO["invcnt"] + (g + 1) * 16] = 1.0 / np.minimum(np.arange(16) + 1.0, float(w))
    C[:, CO["ident"]:CO["ident"] + 128] = np.eye(128)
    C[p, CO["pswap"] + (p ^ 32)] = 1.0
    C[:, CO["onesd"]:CO["onesd"] + 128] = 1.0 / 1024.0
    C[:, CO["onesh"]:CO["onesh"] + 128] = 1.0 / 128.0
    C = C.astype(np.float32)
    half = 32
    inv_freq = (10000.0 ** (-(np.arange(half, dtype=np.float32) / half))).astype(np.float32)
    ang = (np.arange(SEQ, dtype=np.float32)[None, :] * inv_freq[:, None]).astype(np.float32)
    cos = np.cos(ang.astype(np.float64))
    sin = np.sin(ang.astype(np.float64))
    CS = np.zeros((128, 2, SEQ), np.float32)
    for pp in range(128):
        f = pp % 32
        CS[pp, 0] = cos[f]
        CS[pp, 1] = -sin[f] if (pp % 64) < 32 else sin[f]
    return C, CS


PO_ = {}
_c = 0
for _name, _n in (("bada", DEPTH * 48), ("norm1", DEPTH * 8), ("norm2", DEPTH * 8),
                  ("pscale", DEPTH * 4), ("fnorm", 8)):
    PO_[_name] = _c
    _c += _n
NPAR = _c


class Buf:
    __slots__ = ("name", "w", "r", "excl")

    def __init__(self, name, excl=False):
        self.name = name
        self.w = None
        self.r = {}
        self.excl = excl


class Op:
    __slots__ = ("eng", "fn", "deps", "sig", "sem", "val", "is_dma", "dsem", "batch")


class DSem:
    def __init__(self):
        self.h = None
        self.count = 0


class Prog:
    ENGS = ("pe", "act", "dve", "pool", "sp")

    def __init__(self):
        self.q = {e: [] for e in self.ENGS}
        self.dsems = []
        self.nops = 0

    def dsem(self):
        s = DSem()
        self.dsems.append(s)
        return s

    def _add(self, eng, fn, reads, writes, is_dma, dsem, batch):
        o = Op()
        o.eng = eng
        o.fn = fn
        o.sig = False
        o.sem = None
        o.val = 0
        o.is_dma = is_dma
        o.dsem = dsem
        o.batch = batch
        deps = {}

        def add(d):
            if d is None:
                return
            if (not d.is_dma) and (not is_dma) and d.eng == "pe" and eng == "pe":
                return
            deps[id(d)] = d

        for b in reads:
            add(b.w)
            if b.excl:
                for k_, x in b.r.items():
                    if k_ != eng:
                        add(x)
        for b in writes:
            add(b.w)
            for x in b.r.values():
                add(x)
        o.deps = list(deps.values())
        for d in o.deps:
            d.sig = True
        if is_dma:
            o.sig = True
        key = ("dma", id(o)) if is_dma else eng
        for b in writes:
            b.w = o
            b.r = {}
        for b in reads:
            b.r[key] = o
        self.q[eng].append(o)
        self.nops += 1
        return o

    def op(self, eng, fn, reads=(), writes=()):
        return self._add(eng, fn, reads, writes, False, None, None)

    def dma(self, eng, fn, reads, writes, dsem, batch=None):
        o = self._add(eng, fn, reads, writes, True, dsem, batch)
        if batch is not None:
            batch.append(o)
        return o

    def emit(self, nc, stack):
        esems = {}
        for e in self.ENGS:
            n = sum(1 for o in self.q[e] if o.sig and not o.is_dma)
            esems[e] = [stack.enter_context(nc.semaphore(f"s_{e}_{i}")) for i in range(n // EPOCH + 1)]
            c = 0
            for o in self.q[e]:
                if o.sig and not o.is_dma:
                    o.sem = esems[e][c // EPOCH]
                    o.val = c % EPOCH + 1
                    c += 1
        for i, s in enumerate(self.dsems):
            s.h = stack.enter_context(nc.semaphore(f"s_dma_{i}"))
        done_batches = set()
        for e in self.ENGS:
            for o in self.q[e]:
                if not o.is_dma:
                    continue
                o.sem = o.dsem.h
                if o.batch is None:
                    o.dsem.count += 16
                    o.val = o.dsem.count
                elif id(o.batch) not in done_batches:
                    done_batches.add(id(o.batch))
                    fin = o.dsem.count + 16 * len(o.batch)
                    o.dsem.count = fin
                    for x in o.batch:
                        x.val = fin
        block = stack.enter_context(nc.Block())

        def run(eng_name):
            def body(eng):
                seen = {}
                for o in self.q[eng_name]:
                    for d in o.deps:
                        k = id(d.sem)
                        if seen.get(k, 0) >= d.val:
                            continue
                        seen[k] = d.val
                        eng.wait_ge(d.sem, d.val)
                    ins = o.fn(eng)
                    if o.sig:
                        ins.then_inc(o.sem, 16 if o.is_dma else 1)
            return body

        block.tensor(run("pe"))
        block.scalar(run("act"))
        block.vector(run("dve"))
        block.gpsimd(run("pool"))
        block.sync(run("sp"))


def build_program(n_layers=DEPTH, passes=((0, 0), (0, 1), (1, 0), (1, 1))):
    nc = bass.Bass("TRN2", target_bir_lowering=False)
    P = Prog()
    stack = contextlib.ExitStack()
    n_blocks_total = n_layers * NB_LAYER

    xT = nc.dram_tensor("xT", [2, 8, 128, SEQ], F32, kind="ExternalInput").ap()
    cT = nc.dram_tensor("cT", [128, 8, 2], F32, kind="ExternalInput").ap()
    wst = nc.dram_tensor("wst", [DEPTH * NB_LAYER, 128, BLK], F32, kind="ExternalInput").ap()
    wada = nc.dram_tensor("wada", [DEPTH * 12, 128, 8 * 512], F32, kind="ExternalInput").ap()
    par = nc.dram_tensor("par", [128, NPAR], F32, kind="ExternalInput").ap()
    cst = nc.dram_tensor("cst", [128, NCONST], F32, kind="ExternalInput").ap()
    csd = nc.dram_tensor("csd", [128, 2, SEQ], F32, kind="ExternalInput").ap()
    outT = nc.dram_tensor("outT", [2, 8, 128, SEQ], F32, kind="ExternalOutput").ap()

    def sb(name, shape, dt):
        return stack.enter_context(nc.sbuf_tensor(name, shape, dt))

    x_sb = sb("x_sb", [128, 8, TT], F32)
    h_sb = sb("h_sb", [128, 8, TT], BF16)
    act_sb = sb("act_sb", [128, NFF, TT], BF16)
    rg_sb = sb("rg_sb", [128, 8, TT], BF16)
    scr_sb = sb("scr_sb", [128, NSCR, SCRW], F32)
    pl_sb = sb("pl_sb", [128, 3, 16 + TT], F32)
    cs_sb = sb("cs_sb", [128, 2, TT], F32)
    cst_sb = sb("cst_sb", [128, NCONST], F32)
    par_sb = sb("par_sb", [128, NPAR], F32)
    mod_sb = sb("mod_sb", [128, DEPTH, 48, 2], F32)
    a_sb = sb("a_sb", [128, DEPTH, 2, 8, 2], F32)
    cact_sb = sb("cact_sb", [128, 8, 2], F32)
    cbf_sb = sb("cbf_sb", [128, 2, 128], BF16)
    stg_sb = sb("stg_sb", [128, NS, BLK], F32)
    wbf_sb = sb("wbf_sb", [128, NW, BLK], BF16)
    st32_sb = sb("st32_sb", [128, DEPTH, 4, 128], F32)
    stb_sb = sb("stb_sb", [128, DEPTH, 2, 4, 128], BF16)
    carry_sb = sb("carry_sb", [128, DEPTH, 4, 16], F32)
    ps_t = [stack.enter_context(nc.psum_tensor(f"ps{i}", [128, 512], F32)) for i in range(8)]

    X = [[Buf(f"x{j}{s}") for s in range(NSUB)] for j in range(8)]
    H = [[Buf(f"h{j}{s}") for s in range(NSUB)] for j in range(8)]
    A = [[Buf(f"a{i}{s}") for s in range(NSUB)] for i in range(NFF)]
    RG = [[Buf(f"rg{j}{s}") for s in range(NSUB)] for j in range(8)]
    SCR = [Buf(f"scr{i}") for i in range(NSCR)]
    PL = [Buf(f"pl{i}") for i in range(3)]
    CSB = Buf("cs")
    CSTB = Buf("cst")
    PARB = Buf("par")
    MODB = Buf("mod")
    CACTB = Buf("cact")
    CBFB = Buf("cbf")
    STG = [Buf(f"stg{i}") for i in range(NS)]
    WBF = [Buf(f"wbf{i}") for i in range(NW)]
    ST32 = [Buf(f"st32_{l}") for l in range(DEPTH)]
    STB = [[Buf(f"stb_{l}_{i}") for i in range(2)] for l in range(DEPTH)]
    CARRY = [[Buf(f"carry_{l}_{g}") for g in range(4)] for l in range(DEPTH)]
    PS = [Buf(f"psb{i}", excl=True) for i in range(8)]

    state = {"scr": 0, "ps": 0}

    def scr():
        i = state["scr"]
        state["scr"] = (i + 1) % NSCR
        return scr_sb[:, i, :], SCR[i]

    def psum():
        i = state["ps"]
        state["ps"] = (i + 1) % 8
        return ps_t[i][:], PS[i]

    def cview(name, n):
        return cst_sb[:, CO[name]:CO[name] + n]

    class WS:
        def __init__(self):
            self.next_dma = 0
            self.next_cast = 0
            self.maxb = -1
            self.sems = [P.dsem() for _ in range(NS)]
            self.order = []
            self.ncast = 0

        def set_order(self, order):
            self.order = order

        def _dma(self, b):
            if b >= len(self.order):
                return
            slot = b % NS
            src = wst[self.order[b]]
            P.dma("sp", lambda e, slot=slot, src=src: e.dma_start(out=stg_sb[:, slot, :], in_=src),
                  reads=[], writes=[STG[slot]], dsem=self.sems[slot])

        def _cast(self, b):
            while self.next_dma < b + NS:
                self._dma(self.next_dma)
                self.next_dma += 1
            ss, ws = b % NS, b % NW
            eng = ("act", "dve")[self.ncast % 2]
            self.ncast += 1
            if eng == "dve":
                P.op("dve", lambda e, ss=ss, ws=ws: e.tensor_copy(out=wbf_sb[:, ws, :], in_=stg_sb[:, ss, :]),
                     reads=[STG[ss]], writes=[WBF[ws]])
            elif eng == "pool":
                P.op("pool", lambda e, ss=ss, ws=ws: e.tensor_copy(out=wbf_sb[:, ws, :], in_=stg_sb[:, ss, :]),
                     reads=[STG[ss]], writes=[WBF[ws]])
            else:
                P.op("act", lambda e, ss=ss, ws=ws: e.copy(out=wbf_sb[:, ws, :], in_=stg_sb[:, ss, :]),
                     reads=[STG[ss]], writes=[WBF[ws]])
            if self.next_dma == b + NS:
                self._dma(self.next_dma)
                self.next_dma += 1

        def touch(self, b):
            tgt = min(b + LOOKAHEAD, len(self.order) - 1)
            while self.next_cast <= tgt:
                self._cast(self.next_cast)
                self.next_cast += 1
            if b > self.maxb:
                self.maxb = b
            assert b > self.maxb - WINDOW, (b, self.maxb)

        def tile(self, base_blk, t, n=1):
            b = base_blk + t // BLK_TILES
            assert (t % BLK_TILES) + n <= BLK_TILES
            self.touch(b)
            ws = b % NW
            c0 = (t % BLK_TILES) * 128
            return wbf_sb[:, ws, c0:c0 + 128 * n], WBF[ws]

    ws = WS()
    order = []
    for (sq, hf) in passes:
        for l in range(n_layers):
            order.extend(range(l * NB_LAYER, (l + 1) * NB_LAYER))
    ws.set_order(order)

    ld_sem = P.dsem()
    ldb = []
    P.dma("sp", lambda e: e.dma_start(out=cst_sb[:], in_=cst[:, :]), [], [CSTB], ld_sem, ldb)
    P.dma("sp", lambda e: e.dma_start(out=par_sb[:], in_=par[:, :]), [], [PARB], ld_sem, ldb)
    P.dma("sp", lambda e: e.dma_start(out=cact_sb[:], in_=cT[:, :, :]), [], [CACTB], ld_sem, ldb)
    P.op("dve", lambda e: e.tensor_copy(out=cbf_sb[:, 0, :], in_=cview("ident", 128)), [CSTB], [CBFB])
    P.op("dve", lambda e: e.tensor_copy(out=cbf_sb[:, 1, :], in_=cview("pswap", 128)), [CBFB, CSTB], [CBFB])
    P.op("act", lambda e: e.activation(out=cact_sb[:], in_=cact_sb[:], func=AF.Silu), [CACTB], [CACTB])
    ident = cbf_sb[:, 0, :]
    pswap = cbf_sb[:, 1, :]
    onesd = cview("onesd", 128)
    onesh = cview("onesh", 128)

    wa_f32 = act_sb[:].rearrange("p a b -> p (a b)").bitcast(F32)
    WA = [Buf("wa0"), Buf("wa1")]
    wa_sems = [P.dsem(), P.dsem()]
    allA = [A[i][s] for i in range(NFF) for s in range(NSUB)]
    for l in range(n_layers):
        mps, mpsb = psum()
        for pc in range(12):
            slot = (l * 12 + pc) % 2
            src = wada[l * 12 + pc]
            P.dma("sp", lambda e, slot=slot, src=src: e.dma_start(out=wa_f32[:, slot * 4096:(slot + 1) * 4096], in_=src),
                  [], [WA[slot]], wa_sems[slot])
            for ct in range(4):
                t = pc * 4 + ct
                for kk in range(8):
                    P.op("pe", lambda e, slot=slot, ct=ct, kk=kk, t=t, mps=mps: e.matmul(
                        mps[:, 2 * t:2 * t + 2],
                        wa_f32[:, slot * 4096 + kk * 512 + ct * 128: slot * 4096 + kk * 512 + ct * 128 + 128],
                        cact_sb[:, kk, :], start=(kk == 0), stop=(kk == 7)),
                        [WA[slot], CACTB], [mpsb])
        bada = par_sb[:, PO_["bada"] + l * 48: PO_["bada"] + (l + 1) * 48]
        P.op("dve", lambda e, l=l, mps=mps, bada=bada: e.tensor_tensor(
            out=mod_sb[:, l, :, :], in0=mps[:, 0:96].rearrange("p (t b) -> p t b", b=2),
            in1=bada.unsqueeze(2).broadcast_to([128, 48, 2]), op=ALU.add), [mpsb, PARB], [MODB])
        for k, (t0, nm) in enumerate(((8, "norm1"), (32, "norm2"))):
            nv = par_sb[:, PO_[nm] + l * 8: PO_[nm] + (l + 1) * 8]
            P.op("dve", lambda e, l=l, k=k, t0=t0, nv=nv: e.scalar_tensor_tensor(
                out=a_sb[:, l, k, :, :], in0=mod_sb[:, l, t0:t0 + 8, :], scalar=1.0,
                in1=nv.unsqueeze(2).broadcast_to([128, 8, 2]), op0=ALU.add, op1=ALU.mult), [MODB, PARB], [MODB])
    P.op("pool", lambda e: e.memset(act_sb[:, 0, 0:16], 0.0), [], allA + WA)

    def modv(l, t, b):
        return mod_sb[:, l, t, b:b + 1]

    def rmsnorm_to_h(l, bsel, which):
        tB = 0 if which == 0 else 24
        for s in range(NSUB):
            sl = slice(s * 512, (s + 1) * 512)
            stp, stb_ = psum()
            for j in range(8):
                sq, sqb = scr()
                P.op("act", lambda e, j=j, sq=sq, sl=sl: e.activation(out=sq[:, 0:512], in_=x_sb[:, j, sl], func=AF.Square),
                     [X[j][s]], [sqb])
                P.op("pe", lambda e, j=j, sq=sq, stp=stp: e.matmul(stp, onesd, sq[:, 0:512], start=(j == 0), stop=(j == 7)),
                     [sqb, CSTB], [stb_])
            rs, rsb = scr()
            P.op("act", lambda e, rs=rs, stp=stp: e.activation(out=rs[:, 0:512], in_=stp, func=AF.Sqrt, bias=EPS, scale=1.0),
                 [stb_], [rsb])
            P.op("dve", lambda e, rs=rs: e.reciprocal(out=rs[:, 0:512], in_=rs[:, 0:512]), [rsb], [rsb])
            for j in range(8):
                tm, tmb = scr()
                P.op("dve", lambda e, j=j, tm=tm, rs=rs, sl=sl: e.tensor_tensor(
                    out=tm[:, 0:512], in0=x_sb[:, j, sl], in1=rs[:, 0:512], op=ALU.mult), [X[j][s], rsb], [tmb])
                P.op("act", lambda e, j=j, tm=tm, sl=sl: e.activation(
                    out=h_sb[:, j, sl], in_=tm[:, 0:512], func=AF.Identity,
                    bias=modv(l, tB + j, bsel), scale=a_sb[:, l, which, j, bsel:bsel + 1]),
                    [tmb, MODB], [H[j][s]])

    def proj_fm(base, t0, s, nk=8, rhs_of=None):
        ps, psb = psum()
        sl = slice(s * 512, (s + 1) * 512)
        for kk in range(nk):
            w, wb = ws.tile(base, t0 + kk)
            r, rb = rhs_of(kk, s, sl)
            P.op("pe", lambda e, ps=ps, w=w, r=r, kk=kk: e.matmul(ps, w, r, start=(kk == 0), stop=(kk == nk - 1)),
                 [wb, rb], [psb])
        return ps, psb

    def rhs_h(kk, s, sl):
        return h_sb[:, kk, sl], H[kk][s]

    def rhs_rg(kk, s, sl):
        return rg_sb[:, kk, sl], RG[kk][s]

    def rhs_a(off):
        def f(kk, s, sl):
            return act_sb[:, off + kk, sl], A[off + kk][s]
        return f

    def layer(l, bsel, hf, base):
        pos0 = hf * TT
        rmsnorm_to_h(l, bsel, 0)

        if DEBUG_STOP == 1:
            return
        def rot_stage2(part, j, s, sl, rawbf, rawb, t1, t1b):
            ps2, ps2b = psum()
            P.op("pe", lambda e: e.matmul(ps2, pswap, rawbf, start=True, stop=True), [rawb, CBFB], [ps2b])
            t2, t2b = scr()
            P.op("dve", lambda e: e.tensor_tensor(out=t2[:, 0:512], in0=ps2, in1=cs_sb[:, 1, sl], op=ALU.mult),
                 [ps2b, CSB], [t2b])
            if part == 1:
                P.op("pool", lambda e: e.tensor_tensor(
                    out=act_sb[:, 4 + j, sl], in0=t1[:, 0:512], in1=t2[:, 0:512], op=ALU.add),
                    [t1b, t2b], [A[4 + j][s]])
            else:
                P.op("pool", lambda e: e.tensor_tensor(
                    out=t1[:, 0:512], in0=t1[:, 0:512], in1=t2[:, 0:512], op=ALU.add), [t1b, t2b], [t1b])
                qd = cst_sb[:, CO["qdec"] + j * 128: CO["qdec"] + (j + 1) * 128]
                P.op("pool", lambda e: e.tensor_tensor(
                    out=act_sb[:, j, sl].rearrange("p (a b) -> p a b", a=4),
                    in0=t1[:, 0:512].rearrange("p (a b) -> p a b", a=4),
                    in1=qd.unsqueeze(1).broadcast_to([128, 4, 128]), op=ALU.mult),
                    [t1b, CSTB], [A[j][s]])

        pending = None
        for part in range(2):
            for j in range(4):
                for s in range(NSUB):
                    sl = slice(s * 512, (s + 1) * 512)
                    ps, psb = proj_fm(base, OFF["q" if part == 0 else "k"] + j * 8, s, 8, rhs_h)
                    raw, rawb = scr()
                    rawbf = raw.bitcast(BF16)[:, 0:512]
                    P.op("act", lambda e, rawbf=rawbf, ps=ps: e.copy(out=rawbf, in_=ps), [psb], [rawb])
                    t1, t1b = scr()
                    P.op("dve", lambda e, t1=t1, ps=ps, sl=sl: e.tensor_tensor(
                        out=t1[:, 0:512], in0=ps, in1=cs_sb[:, 0, sl], op=ALU.mult), [psb, CSB], [t1b])
                    if pending is not None:
                        rot_stage2(*pending)
                    pending = (part, j, s, sl, rawbf, rawb, t1, t1b)
        rot_stage2(*pending)

        kdec = cview("kdec", 8)

        def k_transpose(n):
            s, c0 = n // 4, (n % 4) * 128
            ps, psb = psum()
            psbf = ps.bitcast(BF16)
            for j in range(4):
                P.op("pe", lambda e, j=j: e.transpose(
                    psbf[:, j * 128:(j + 1) * 128], act_sb[:, 4 + j, s * 512 + c0: s * 512 + c0 + 128], ident),
                    [A[4 + j][s], CBFB], [psb])
            P.op("dve", lambda e: e.tensor_tensor(
                out=act_sb[:, 16 + n // 2, (n % 2) * 512:(n % 2 + 1) * 512].rearrange("p (h a) -> p h a", h=8),
                in0=psbf[:, 0:512].rearrange("p (h a) -> p h a", h=8),
                in1=kdec.unsqueeze(2).broadcast_to([128, 8, 64]), op=ALU.mult),
                [psb, CSTB], [A[16 + n // 2][n % 2]])

        gi = 0
        for cg in range(2):
            for n in range(NCH):
                s, c0 = n // 4, (n % 4) * 128
                ps, psb = psum()
                for kk in range(8):
                    w, wb = ws.tile(base, OFF["v"] + (cg * 8 + kk) * 4, 4)
                    P.op("pe", lambda e, ps=ps, w=w, kk=kk, s=s, c0=c0: e.matmul(
                        ps, h_sb[:, kk, s * 512 + c0: s * 512 + c0 + 128], w, start=(kk == 0), stop=(kk == 7)),
                        [wb, H[kk][s]], [psb])
                P.op("dve", lambda e, ps=ps, n=n, cg=cg: e.tensor_copy(out=act_sb[:, 8 + n, cg * 512:(cg + 1) * 512], in_=ps),
                     [psb], [A[8 + n][cg]])
                if gi % 2 == 1:
                    k_transpose(gi // 2)
                gi += 1

        if hf == 0:
            P.op("pool", lambda e: e.memset(st32_sb[:, l, :, :], 0.0), [], [ST32[l]])
            P.op("pool", lambda e: e.memset(stb_sb[:, l, 0, :, :], 0.0), [], [STB[l][0]])

        mask = cview("mask", 1024)
        cdec = cview("cdec", 4)
        mask4 = mask.rearrange("p (j q c) -> p j q c", j=4, q=2)
        rg4 = rg_sb[:].rearrange("p (j q) t -> p j q t", q=2)
        for n in range(NCH):
            s, c0 = n // 4, (n % 4) * 128
            tsl = slice(s * 512 + c0, s * 512 + c0 + 128)
            vbufs = [A[8 + n][0], A[8 + n][1]]
            cur, nxt = n % 2, (n + 1) % 2
            sc_ps = [psum(), psum()]
            for h in range(8):
                j, par = h // 2, h % 2
                p0 = par * 64
                bank, bankb = sc_ps[par]
                P.op("pe", lambda e, bank=bank, j=j, p0=p0, tsl=tsl: e.matmul(
                    bank[:, j * 128:(j + 1) * 128], act_sb[p0:p0 + 64, 4 + j, tsl], act_sb[p0:p0 + 64, j, tsl],
                    start=True, stop=True), [A[4 + j][s], A[j][s]], [bankb])
            pkv, pkvb = psum()
            kdb = A[16 + n // 2][n % 2]
            for h in range(8):
                P.op("pe", lambda e, pkv=pkv, h=h, n=n: e.matmul(
                    pkv[(h % 2) * 64:(h % 2) * 64 + 64, (h // 2) * 128:(h // 2 + 1) * 128],
                    act_sb[:, 16 + n // 2, (n % 2) * 512 + h * 64:(n % 2) * 512 + (h + 1) * 64],
                    act_sb[:, 8 + n, h * 128:(h + 1) * 128], start=True, stop=True),
                    [kdb, vbufs[h // 4]], [pkvb])
            stts = []
            for par in range(2):
                bank, bankb = sc_ps[par]
                stt, sttb = scr()
                sttbf = stt.bitcast(BF16)[:, 0:512]
                P.op("dve", lambda e, sttbf=sttbf, bank=bank, par=par: e.tensor_tensor(
                    out=sttbf.rearrange("p (j c) -> p j c", j=4), in0=bank.rearrange("p (j c) -> p j c", j=4),
                    in1=mask4[:, :, par, :], op=ALU.mult), [bankb, CSTB], [sttb])
                stts.append((sttbf, sttb))
            tmp, tmpb = scr()
            P.op("pool", lambda e, tmp=tmp: e.tensor_tensor(
                out=tmp[:, 0:512].rearrange("p (a b) -> p a b", a=4), in0=st32_sb[:, l, :, :],
                in1=cdec.unsqueeze(2).broadcast_to([128, 4, 128]), op=ALU.mult), [ST32[l], CSTB], [tmpb])
            P.op("dve", lambda e, tmp=tmp, pkv=pkv: e.tensor_tensor(
                out=st32_sb[:, l, :, :], in0=tmp[:, 0:512].rearrange("p (a b) -> p a b", a=4),
                in1=pkv.rearrange("p (a b) -> p a b", a=4), op=ALU.add), [tmpb, pkvb], [ST32[l]])
            P.op("act", lambda e, nxt=nxt: e.copy(out=stb_sb[:, l, nxt, :, :], in_=st32_sb[:, l, :, :]),
                 [ST32[l]], [STB[l][nxt]])
            o_ps = [psum(), psum()]
            for h in range(8):
                j, par = h // 2, h % 2
                p0 = par * 64
                bank, bankb = o_ps[par]
                sttbf, sttb = stts[par]
                P.op("pe", lambda e, bank=bank, j=j, h=h, n=n, sttbf=sttbf: e.matmul(
                    bank[:, j * 128:(j + 1) * 128], act_sb[:, 8 + n, h * 128:(h + 1) * 128],
                    sttbf[:, j * 128:(j + 1) * 128], start=True, stop=False),
                    [vbufs[h // 4], sttb], [bankb])
                P.op("pe", lambda e, bank=bank, j=j, p0=p0, tsl=tsl, cur=cur: e.matmul(
                    bank[:, j * 128:(j + 1) * 128], stb_sb[p0:p0 + 64, l, cur, j, :],
                    act_sb[p0:p0 + 64, j, tsl], start=False, stop=True),
                    [STB[l][cur], A[j][s]], [bankb])
            for par in range(2):
                bank, bankb = o_ps[par]
                osq, osqb = scr()
                P.op("act", lambda e, osq=osq, bank=bank: e.activation(out=osq[:, 0:512], in_=bank, func=AF.Square),
                     [bankb], [osqb])
                psn, psnb = psum()
                P.op("pe", lambda e, psn=psn, osq=osq: e.matmul(psn, onesh, osq[:, 0:512], start=True, stop=True),
                     [osqb, CSTB], [psnb])
                hr, hrb = scr()
                P.op("act", lambda e, hr=hr, psn=psn: e.activation(out=hr[:, 0:512], in_=psn, func=AF.Sqrt, bias=EPS, scale=1.0),
                     [psnb], [hrb])
                P.op("dve", lambda e, hr=hr: e.reciprocal(out=hr[:, 0:512], in_=hr[:, 0:512]), [hrb], [hrb])
                P.op("dve", lambda e, hr=hr, bank=bank, par=par, tsl=tsl: e.tensor_tensor(
                    out=rg4[:, :, par, tsl], in0=bank.rearrange("p (a b) -> p a b", a=4),
                    in1=hr[:, 0:512].rearrange("p (a b) -> p a b", a=4), op=ALU.mult),
                    [hrb, bankb], [RG[2 * i + par][s] for i in range(4)])

        if DEBUG_STOP == 5:
            return
        for h in range(8):
            for s in range(NSUB):
                sl = slice(s * 512, (s + 1) * 512)
                ps, psb = proj_fm(base, OFF["g"] + h * 8, s, 8, rhs_h)
                sg, sgb = scr()
                sgbf = sg.bitcast(BF16)[:, 0:512]
                P.op("act", lambda e, sgbf=sgbf, ps=ps: e.activation(out=sgbf, in_=ps, func=AF.Silu), [psb], [sgb])
                P.op("pool", lambda e, h=h, sl=sl, sgbf=sgbf: e.tensor_tensor(
                    out=rg_sb[:, h, sl], in0=rg_sb[:, h, sl], in1=sgbf, op=ALU.mult), [sgb, RG[h][s]], [RG[h][s]])

        if DEBUG_STOP == 6:
            return
        invc = cview("invcnt", 64)
        for g in range(4):
            w = 2 << g
            for s in range(NSUB):
                ps, psb = proj_fm(base, OFF["p"] + g * 8, s, 8, rhs_h)
                P.op("act", lambda e, ps=ps, s=s: e.copy(out=pl_sb[:, 0, 16 + s * 512:16 + (s + 1) * 512], in_=ps),
                     [psb], [PL[0]])
            if hf == 0:
                P.op("pool", lambda e: e.memset(pl_sb[:, 0, 0:16], 0.0), [], [PL[0]])
            else:
                P.op("pool", lambda e, g=g: e.tensor_copy(out=pl_sb[:, 0, 0:16], in_=carry_sb[:, l, g, :]),
                     [CARRY[l][g]], [PL[0]])
            P.op("pool", lambda e, g=g: e.tensor_copy(out=carry_sb[:, l, g, :], in_=pl_sb[:, 0, TT:TT + 16]),
                 [PL[0]], [CARRY[l][g]])
            cur, curb = 0, PL[0]
            sh, lo = 1, 0
            while sh < w:
                nxt = 1 if cur != 1 else 2
                lo2 = lo + sh
                P.op("pool", lambda e, cur=cur, nxt=nxt, sh=sh, lo2=lo2: e.tensor_tensor(
                    out=pl_sb[:, nxt, lo2:16 + TT], in0=pl_sb[:, cur, lo2:16 + TT],
                    in1=pl_sb[:, cur, lo2 - sh:16 + TT - sh], op=ALU.add), [PL[cur]], [PL[nxt]])
                cur, lo, sh = nxt, lo2, sh * 2
            P.op("dve", lambda e, cur=cur, g=g, w=w: e.scalar_tensor_tensor(
                out=act_sb[:, 8 + g, :], in0=pl_sb[:, cur, 16:16 + TT], scalar=1.0 / w,
                in1=pl_sb[:, 0, 16:16 + TT], op0=ALU.mult, op1=ALU.subtract), [PL[cur], PL[0]], [A[8 + g][0], A[8 + g][1]])
            if hf == 0:
                tm, tmb = scr()
                P.op("dve", lambda e, tm=tm, cur=cur, g=g: e.tensor_tensor(
                    out=tm[:, 0:16], in0=pl_sb[:, cur, 16:32], in1=invc[:, g * 16:(g + 1) * 16], op=ALU.mult),
                    [PL[cur], CSTB], [tmb])
                P.op("dve", lambda e, tm=tm, g=g: e.tensor_tensor(
                    out=act_sb[:, 8 + g, 0:16], in0=tm[:, 0:16], in1=pl_sb[:, 0, 16:32], op=ALU.subtract),
                    [tmb, PL[0], A[8 + g][0]], [A[8 + g][0]])
        for g in range(4):
            for s in range(NSUB):
                sl = slice(s * 512, (s + 1) * 512)
                ps, psb = psum()
                wt, wtb = ws.tile(base, OFF["grp"] + g)
                P.op("pe", lambda e, ps=ps, wt=wt, g=g, sl=sl: e.matmul(ps, wt, act_sb[:, 8 + g, sl], start=True, stop=True),
                     [wtb, A[8 + g][s]], [psb])
                psc = par_sb[:, PO_["pscale"] + l * 4 + g: PO_["pscale"] + l * 4 + g + 1]
                P.op("act", lambda e, ps=ps, g=g, sl=sl, psc=psc: e.activation(
                    out=act_sb[:, 12 + g, sl], in_=ps, func=AF.Identity, bias=0.0, scale=psc), [psb, PARB], [A[12 + g][s]])

        if DEBUG_STOP == 7:
            return
        for j in range(8):
            m0 = OFF["mrg"] + j * 28
            for s in range(NSUB):
                sl = slice(s * 512, (s + 1) * 512)
                prd, prdb = proj_fm(base, m0, s, 8, rhs_rg)
                ppd, ppdb = proj_fm(base, m0 + 8, s, 4, rhs_a(12))
                par_, parb_ = proj_fm(base, m0 + 12, s, 8, rhs_h)
                pap, papb = proj_fm(base, m0 + 20, s, 8, rhs_h)
                sr, srb = scr()
                P.op("act", lambda e, sr=sr, par_=par_: e.activation(out=sr[:, 0:512], in_=par_, func=AF.Sigmoid), [parb_], [srb])
                sp_, spb = scr()
                P.op("act", lambda e, sp_=sp_, pap=pap: e.activation(out=sp_[:, 0:512], in_=pap, func=AF.Sigmoid), [papb], [spb])
                P.op("dve", lambda e, sr=sr, prd=prd: e.tensor_tensor(out=sr[:, 0:512], in0=prd, in1=sr[:, 0:512], op=ALU.mult),
                     [prdb, srb], [srb])
                P.op("dve", lambda e, sp_=sp_, ppd=ppd: e.tensor_tensor(out=sp_[:, 0:512], in0=ppd, in1=sp_[:, 0:512], op=ALU.mult),
                     [ppdb, spb], [spb])
                P.op("pool", lambda e, j=j, sl=sl, sr=sr, sp_=sp_: e.tensor_tensor(
                    out=act_sb[:, j, sl], in0=sr[:, 0:512], in1=sp_[:, 0:512], op=ALU.add), [srb, spb], [A[j][s]])

        if DEBUG_STOP == 8:
            return
        for j in range(8):
            for s in range(NSUB):
                sl = slice(s * 512, (s + 1) * 512)
                ps, psb = proj_fm(base, OFF["out"] + j * 8, s, 8, rhs_a(0))
                P.op("dve", lambda e, ps=ps, j=j, sl=sl: e.scalar_tensor_tensor(
                    out=x_sb[:, j, sl], in0=ps, scalar=modv(l, 16 + j, bsel), in1=x_sb[:, j, sl],
                    op0=ALU.mult, op1=ALU.add), [psb, MODB, X[j][s]], [X[j][s]])

        if DEBUG_STOP == 9:
            return
        rmsnorm_to_h(l, bsel, 1)
        for i in range(NFF):
            for s in range(NSUB):
                sl = slice(s * 512, (s + 1) * 512)
                pg, pgb = proj_fm(base, OFF["ffi"] + i * 16, s, 8, rhs_h)
                pu, pub = proj_fm(base, OFF["ffi"] + i * 16 + 8, s, 8, rhs_h)
                sg, sgb = scr()
                P.op("act", lambda e, sg=sg, pg=pg: e.activation(out=sg[:, 0:512], in_=pg, func=AF.Silu), [pgb], [sgb])
                P.op("dve", lambda e, sg=sg, pu=pu, i=i, sl=sl: e.tensor_tensor(
                    out=act_sb[:, i, sl], in0=pu, in1=sg[:, 0:512], op=ALU.mult), [pub, sgb], [A[i][s]])
        for j in range(8):
            for s in range(NSUB):
                sl = slice(s * 512, (s + 1) * 512)
                ps, psb = proj_fm(base, OFF["ffo"] + j * NFF, s, NFF, rhs_a(0))
                P.op("dve", lambda e, ps=ps, j=j, sl=sl: e.scalar_tensor_tensor(
                    out=x_sb[:, j, sl], in0=ps, scalar=modv(l, 40 + j, bsel), in1=x_sb[:, j, sl],
                    op0=ALU.mult, op1=ALU.add), [psb, MODB, X[j][s]], [X[j][s]])

    xl_sem = P.dsem()
    st_sem = P.dsem()
    cs_sem = P.dsem()
    store_ops = []
    blk = 0
    for (sq, hf) in passes:
        pos0 = hf * TT
        lb = []
        for j in range(8):
            for s in range(NSUB):
                P.dma("act", lambda e, j=j, s=s, sq=sq, pos0=pos0: e.dma_start(
                    out=x_sb[:, j, s * 512:(s + 1) * 512], in_=xT[sq, j, :, pos0 + s * 512: pos0 + (s + 1) * 512]),
                    [], [X[j][s]], xl_sem, lb)
        P.dma("act", lambda e, pos0=pos0: e.dma_start(out=cs_sb[:], in_=csd[:, :, pos0:pos0 + TT]), [], [CSB], cs_sem)
        for l in range(n_layers):
            layer(l, sq, hf, blk)
            blk += NB_LAYER
        fn = par_sb[:, PO_["fnorm"]: PO_["fnorm"] + 8]
        sbatch = []
        for s in range(NSUB):
            sl = slice(s * 512, (s + 1) * 512)
            stp, stb_ = psum()
            for j in range(8):
                sq_, sqb = scr()
                P.op("act", lambda e, j=j, sq_=sq_, sl=sl: e.activation(out=sq_[:, 0:512], in_=x_sb[:, j, sl], func=AF.Square),
                     [X[j][s]], [sqb])
                P.op("pe", lambda e, j=j, sq_=sq_, stp=stp: e.matmul(stp, onesd, sq_[:, 0:512], start=(j == 0), stop=(j == 7)),
                     [sqb, CSTB], [stb_])
            rs, rsb = scr()
            P.op("act", lambda e, rs=rs, stp=stp: e.activation(out=rs[:, 0:512], in_=stp, func=AF.Sqrt, bias=EPS, scale=1.0),
                 [stb_], [rsb])
            P.op("dve", lambda e, rs=rs: e.reciprocal(out=rs[:, 0:512], in_=rs[:, 0:512]), [rsb], [rsb])
            for j in range(8):
                P.op("dve", lambda e, j=j, rs=rs, sl=sl: e.scalar_tensor_tensor(
                    out=x_sb[:, j, sl], in0=x_sb[:, j, sl], scalar=fn[:, j:j + 1], in1=rs[:, 0:512],
                    op0=ALU.mult, op1=ALU.mult), [X[j][s], rsb, PARB], [X[j][s]])
                o = P.dma("act", lambda e, j=j, s=s, sq=sq, pos0=pos0, sl=sl: e.dma_start(
                    out=outT[sq, j, :, pos0 + s * 512: pos0 + (s + 1) * 512], in_=x_sb[:, j, sl]),
                    [X[j][s]], [], st_sem, sbatch)
                store_ops.append(o)
    fin = P.op("act", lambda e: e.activation(out=scr_sb[:, 0, 0:8], in_=scr_sb[:, 0, 0:8], func=AF.Copy), [], [SCR[0]])
    dd = {id(o): o for o in fin.deps}
    for o in store_ops:
        dd[id(o)] = o
    fin.deps = list(dd.values())

    P.emit(nc, stack)
    return nc, stack, P


def prep_shared(w_ada, b_ada, norm1, w_in, w_ret_o, w_pool_grp, pool_scale, w_pool_o,
                w_out, norm2, w_ffn_in, w_ffn_out, final_norm):
    f = lambda a: np.ascontiguousarray(np.asarray(a, dtype=np.float32))
    wst = np.concatenate([pack_layer(f(w_in[l]), f(w_ret_o[l]), f(w_pool_grp[l]), f(w_pool_o[l]), f(w_out[l]),
                                     f(w_ffn_in[l]), f(w_ffn_out[l])) for l in range(DEPTH)], axis=0)
    wa = f(w_ada).reshape(DEPTH, 8, 128, 12, 512).transpose(0, 3, 2, 1, 4).reshape(DEPTH * 12, 128, 8 * 512)
    par = np.zeros((128, NPAR), np.float32)
    par[:, PO_["bada"]:PO_["bada"] + DEPTH * 48] = f(b_ada).reshape(DEPTH, 48, 128).transpose(2, 0, 1).reshape(128, -1)
    par[:, PO_["norm1"]:PO_["norm1"] + DEPTH * 8] = f(norm1).reshape(DEPTH, 8, 128).transpose(2, 0, 1).reshape(128, -1)
    par[:, PO_["norm2"]:PO_["norm2"] + DEPTH * 8] = f(norm2).reshape(DEPTH, 8, 128).transpose(2, 0, 1).reshape(128, -1)
    par[:, PO_["pscale"]:PO_["pscale"] + DEPTH * 4] = f(pool_scale).reshape(DEPTH, 4, 128).transpose(2, 0, 1).reshape(128, -1)
    par[:, PO_["fnorm"]:PO_["fnorm"] + 8] = f(final_norm).reshape(8, 128).T
    cst, csd = make_consts()
    return {"wst": np.ascontiguousarray(wst), "wada": np.ascontiguousarray(wa), "par": par, "cst": cst, "csd": csd}


def prep_core(x, c, core):
    xs = np.asarray(x[2 * core:2 * core + 2], dtype=np.float32)
    xT = np.ascontiguousarray(xs.transpose(0, 2, 1).reshape(2, 8, 128, SEQ))
    cs = np.asarray(c[2 * core:2 * core + 2], dtype=np.float32)
    cT = np.ascontiguousarray(cs.reshape(2, 8, 128).transpose(2, 1, 0))
    return {"xT": xT, "cT": cT}


_CACHE = {}


def kernel(x, c, w_ada, b_ada, norm1, w_in, w_ret_o, w_pool_grp, pool_scale, w_pool_o,
           w_out, norm2, w_ffn_in, w_ffn_out, final_norm):
    shared = prep_shared(w_ada, b_ada, norm1, w_in, w_ret_o, w_pool_grp, pool_scale, w_pool_o,
                         w_out, norm2, w_ffn_in, w_ffn_out, final_norm)
    nc, stack, P = build_program()
    in_maps = []
    for core in range(NCORES):
        m = dict(shared)
        m.update(prep_core(x, c, core))
        in_maps.append(m)
    res = run_bass_kernel_spmd(nc, in_maps, core_ids=list(range(NCORES)))
    out = np.empty((BATCH, SEQ, D), np.float32)
    for core in range(NCORES):
        o = np.asarray(res.results[core]["outT"]).reshape(2, D, SEQ)
        out[2 * core:2 * core + 2] = o.transpose(0, 2, 1)
    return out
```
